# Optimizing a Trainium2 kernel written in Bass

```python
import jax, jax.numpy as jnp
from jax import lax
import numpy as np

D_MODEL = 2048
BATCH = 8
SEQ = 2048
DEPTH = 1

HEAD_DIM = 128
D_MIX = D_MODEL
N_HEADS_DIL = (D_MIX // 2) // HEAD_DIM
D_DIL = N_HEADS_DIL * HEAD_DIM
DIL_PATTERNS = ((128, 1), (512, 4), (2048, 16))
BLOCK = 128
V_HEAD = 128
N_HEADS_MLA = (D_MIX - D_DIL) // V_HEAD
D_MLA = N_HEADS_MLA * V_HEAD
Q_LORA = 512
KV_LORA = 512
QK_NOPE = 128
QK_ROPE = 64
ROPE_THETA = 10000.0
D_IN = 3 * D_DIL + Q_LORA + KV_LORA + QK_ROPE
IN_SPLITS = [D_DIL, 2 * D_DIL, 3 * D_DIL, 3 * D_DIL + Q_LORA, 3 * D_DIL + Q_LORA + KV_LORA]
PEER_HEADS = 8
PEER_NKEYS = 128
PEER_N = PEER_NKEYS * PEER_NKEYS
PEER_QDIM = 256
PEER_TOPK = 16
PEER_CHUNK = 128
PEER_V_STD = 0.5

EPS = 1e-6
NEG = -1e30
f32 = jnp.float32

kernel_name = "hymba_dilated_mla_peer_block"


def rmsnorm(x, g):
    xf = x.astype(f32)
    y = xf * lax.rsqrt(jnp.mean(xf * xf, axis=-1, keepdims=True) + EPS)
    return (y * g.astype(f32)).astype(x.dtype)


def alibi_slopes(n):
    return 2.0 ** (-8.0 * jnp.arange(1, n + 1, dtype=f32) / n)


def rope(x, pos):
    half = x.shape[-1] // 2
    freqs = ROPE_THETA ** (-jnp.arange(half, dtype=f32) / half)
    ang = pos.astype(f32)[:, None] * freqs[None, :]
    cos, sin = jnp.cos(ang), jnp.sin(ang)
    x1, x2 = x[..., :half].astype(f32), x[..., half:].astype(f32)
    return jnp.concatenate([x1 * cos - x2 * sin, x1 * sin + x2 * cos], axis=-1).astype(x.dtype)


def dilated_window_attn(q, k, v, slopes, window, dilation):
    b, h, s, hd = q.shape
    steps = window // dilation
    L = s // dilation

    def split(t):
        return t.reshape(b, h, L, dilation, hd).transpose(0, 1, 3, 2, 4)

    nb = -(-L // BLOCK)
    pad = nb * BLOCK - L
    padcfg = ((0, 0), (0, 0), (0, 0), (0, pad), (0, 0))
    qs = jnp.pad(split(q), padcfg).reshape(b, h, dilation, nb, BLOCK, hd)
    ks = jnp.pad(split(k), padcfg).reshape(b, h, dilation, nb, BLOCK, hd)
    vs = jnp.pad(split(v), padcfg).reshape(b, h, dilation, nb, BLOCK, hd)

    def with_prev(t):
        prev = jnp.pad(t, ((0, 0), (0, 0), (0, 0), (1, 0), (0, 0), (0, 0)))[:, :, :, :-1]
        return jnp.concatenate([prev, t], axis=4)

    kb, vb = with_prev(ks), with_prev(vs)
    sc = jnp.einsum('bhrnqd,bhrnkd->bhrnqk', qs, kb).astype(f32) * (hd ** -0.5)
    qi = jnp.arange(BLOCK)[:, None]
    kj = jnp.arange(2 * BLOCK)[None, :]
    delta = qi + BLOCK - kj
    key_idx = jnp.arange(nb)[:, None, None] * BLOCK - BLOCK + kj
    valid = (delta >= 0) & (delta <= steps) & (key_idx >= 0)
    bias = -slopes[:, None, None, None, None] * (delta * dilation).astype(f32)
    sc = jnp.where(valid, sc + bias, NEG)
    m = jnp.max(sc, axis=-1, keepdims=True)
    p = jnp.exp(sc - m)
    l = jnp.sum(p, axis=-1, keepdims=True)
    o = jnp.einsum('bhrnqk,bhrnkd->bhrnqd', (p / l).astype(v.dtype), vb)
    lse = (m + jnp.log(l))[..., 0]

    def merge(t):
        t = t.reshape(b, h, dilation, nb * BLOCK, *t.shape[5:])[:, :, :, :L]
        t = jnp.moveaxis(t, 2, 3)
        return t.reshape(b, h, s, *t.shape[4:])

    return merge(o), merge(lse)


def dilated_mixture_attention(q, k, v, slopes):
    res = [dilated_window_attn(q, k, v, slopes, w, d) for (w, d) in DIL_PATTERNS]
    outs = jnp.stack([r[0] for r in res], 0).astype(f32)
    lses = jnp.stack([r[1] for r in res], 0)
    wts = jax.nn.softmax(lses, axis=0)
    return jnp.einsum('pbhs,pbhsd->bhsd', wts, outs).astype(q.dtype)


def causal_block_attention(q, k, v, scale):
    b, h, s, dq = q.shape
    nblk = s // BLOCK
    qb = q.reshape(b, h, nblk, BLOCK, dq).transpose(2, 0, 1, 3, 4)
    kpos = jnp.arange(s)

    def one(args):
        qi, i = args
        sc = jnp.einsum('bhqd,bhkd->bhqk', qi, k).astype(f32) * scale
        qpos = i * BLOCK + jnp.arange(BLOCK)
        sc = jnp.where(kpos[None, :] <= qpos[:, None], sc, NEG)
        p = jax.nn.softmax(sc, axis=-1)
        return jnp.einsum('bhqk,bhkd->bhqd', p.astype(v.dtype), v)

    o = lax.map(one, (qb, jnp.arange(nblk)))
    return o.transpose(1, 2, 0, 3, 4).reshape(b, h, s, v.shape[-1])


def mla_attention(c_q, c_kv, k_rope, q_a_norm, kv_a_norm, w_uq, w_ukv, pos):
    b, s, _ = c_q.shape
    H = N_HEADS_MLA
    q = (rmsnorm(c_q, q_a_norm) @ w_uq).reshape(b, s, H, QK_NOPE + QK_ROPE).transpose(0, 2, 1, 3)
    kv = (rmsnorm(c_kv, kv_a_norm) @ w_ukv).reshape(b, s, H, QK_NOPE + V_HEAD).transpose(0, 2, 1, 3)
    q_nope, q_pe = q[..., :QK_NOPE], q[..., QK_NOPE:]
    k_nope, v = kv[..., :QK_NOPE], kv[..., QK_NOPE:]
    q_pe = rope(q_pe, pos)
    k_pe = jnp.broadcast_to(rope(k_rope, pos)[:, None], (b, H, s, QK_ROPE))
    qf = jnp.concatenate([q_nope, q_pe], axis=-1)
    kf = jnp.concatenate([k_nope, k_pe], axis=-1)
    o = causal_block_attention(qf, kf, v, (QK_NOPE + QK_ROPE) ** -0.5)
    return o.transpose(0, 2, 1, 3).reshape(b, s, D_MLA)


def peer_ffn(xn, w_pq, sub_keys, peer_u, peer_v):
    b, s, d = xn.shape
    T = b * s
    t = xn.reshape(T, d)
    q = (t @ w_pq).reshape(T, PEER_HEADS, 2, PEER_QDIM // 2)
    sc = jnp.einsum('thpc,hpnc->thpn', q, sub_keys).astype(f32)
    top_s, top_i = lax.top_k(sc, PEER_TOPK)
    cand_s = (top_s[:, :, 0, :, None] + top_s[:, :, 1, None, :]).reshape(T, PEER_HEADS, PEER_TOPK * PEER_TOPK)
    cand_i = (top_i[:, :, 0, :, None] * PEER_NKEYS + top_i[:, :, 1, None, :]).reshape(T, PEER_HEADS, PEER_TOPK * PEER_TOPK)
    best_s, best_p = lax.top_k(cand_s, PEER_TOPK)
    idx = jnp.take_along_axis(cand_i, best_p, axis=-1).reshape(T, PEER_HEADS * PEER_TOPK)
    gate = jax.nn.softmax(best_s, axis=-1).reshape(T, PEER_HEADS * PEER_TOPK)
    nchunk = T // PEER_CHUNK

    def expert_chunk(args):
        xc, ic, gc = args
        u = jnp.take(peer_u, ic, axis=0)
        a = jax.nn.gelu(jnp.einsum('cd,ced->ce', xc, u).astype(f32), approximate=False) * gc
        vsel = jnp.take(peer_v, ic, axis=0)
        return jnp.einsum('ce,ced->cd', a.astype(vsel.dtype), vsel)

    y = lax.map(expert_chunk, (t.reshape(nchunk, PEER_CHUNK, d),
                               idx.reshape(nchunk, PEER_CHUNK, -1),
                               gate.reshape(nchunk, PEER_CHUNK, -1)))
    return y.reshape(b, s, d)


def setup_inputs(seed: int = 0) -> dict:
    key = jax.random.key(seed)
    ks = jax.random.split(key, 20)
    nrm = lambda k, shape, std: jax.random.normal(k, shape, f32) * std
    gain = lambda k, shape: 1.0 + 0.02 * jax.random.normal(k, shape, f32)
    return {
        "x": nrm(ks[0], (BATCH, SEQ, D_MODEL), 1.0),
        "ln1_g": gain(ks[1], (DEPTH, D_MODEL)),
        "w_in": nrm(ks[2], (DEPTH, D_MODEL, D_IN), D_MODEL ** -0.5),
        "q_a_norm": gain(ks[3], (DEPTH, Q_LORA)),
        "kv_a_norm": gain(ks[4], (DEPTH, KV_LORA)),
        "w_uq": nrm(ks[5], (DEPTH, Q_LORA, N_HEADS_MLA * (QK_NOPE + QK_ROPE)), Q_LORA ** -0.5),
        "w_ukv": nrm(ks[6], (DEPTH, KV_LORA, N_HEADS_MLA * (QK_NOPE + V_HEAD)), KV_LORA ** -0.5),
        "out_norm_dil": gain(ks[7], (DEPTH, D_DIL)),
        "out_norm_mla": gain(ks[8], (DEPTH, D_MLA)),
        "w_o": nrm(ks[9], (DEPTH, D_MIX, D_MODEL), D_MIX ** -0.5),
        "ln2_g": gain(ks[10], (DEPTH, D_MODEL)),
        "peer_wq": nrm(ks[11], (DEPTH, D_MODEL, PEER_HEADS * PEER_QDIM), D_MODEL ** -0.5),
        "peer_sub_keys": nrm(ks[12], (DEPTH, PEER_HEADS, 2, PEER_NKEYS, PEER_QDIM // 2), (PEER_QDIM // 2) ** -0.5),
        "peer_u": nrm(ks[13], (DEPTH, PEER_N, D_MODEL), D_MODEL ** -0.5),
        "peer_v": nrm(ks[14], (DEPTH, PEER_N, D_MODEL), PEER_V_STD),
        "lnf_g": gain(ks[15], (D_MODEL,)),
    }


def reference(x, ln1_g, w_in, q_a_norm, kv_a_norm, w_uq, w_ukv, out_norm_dil, out_norm_mla, w_o,
              ln2_g, peer_wq, peer_sub_keys, peer_u, peer_v, lnf_g):
    b, s, _ = x.shape
    pos = jnp.arange(s)
    slopes = alibi_slopes(N_HEADS_DIL)
    h = x
    for l in range(DEPTH):
        xn = rmsnorm(h, ln1_g[l])
        z = xn @ w_in[l]
        q_a, k_a, v_a, c_q, c_kv, k_r = jnp.split(z, IN_SPLITS, axis=-1)
        to_heads = lambda t: t.reshape(b, s, N_HEADS_DIL, HEAD_DIM).transpose(0, 2, 1, 3)
        o_a = dilated_mixture_attention(to_heads(q_a), to_heads(k_a), to_heads(v_a), slopes)
        o_a = rmsnorm(o_a.transpose(0, 2, 1, 3).reshape(b, s, D_DIL), out_norm_dil[l])
        o_b = rmsnorm(mla_attention(c_q, c_kv, k_r, q_a_norm[l], kv_a_norm[l], w_uq[l], w_ukv[l], pos),
                      out_norm_mla[l])
        h = h + jnp.concatenate([o_a, o_b], axis=-1) @ w_o[l]
        h = h + peer_ffn(rmsnorm(h, ln2_g[l]), peer_wq[l], peer_sub_keys[l], peer_u[l], peer_v[l])
    return rmsnorm(h, lnf_g)
```

```python
import contextlib
import numpy as np
import concourse.bass as bass
import concourse.mybir as mybir
from concourse.bass_utils import run_bass_kernel_spmd

F32 = mybir.dt.float32
BF16 = mybir.dt.bfloat16
U32 = mybir.dt.uint32
U8 = mybir.dt.uint8
ALU = mybir.AluOpType
AF = mybir.ActivationFunctionType
AX = mybir.AxisListType

S = 2048
DM = 2048
NT = 16
EPS = 1e-6
NEGBIG = -30000.0
KB = 1024

ENGS = ("pe", "act", "dve", "pool", "sp")
NDMASEM = 8


class Op:
    def __init__(self, eng, fn, reads, writes, dma):
        self.eng = eng; self.fn = fn; self.reads = tuple(reads); self.writes = tuple(writes); self.dma = dma
        self.waits = {}; self.has_dep = False; self.seq = None; self.deps = []


class Prog:
    def __init__(self, nc):
        self.nc = nc
        self.ops = []

    def add(self, eng, fn, reads=(), writes=(), dma=0):
        op = Op(eng, fn, reads, writes, dma)
        self.ops.append(op)
        return op

    def pe(self, fn, r=(), w=()): return self.add("pe", fn, r, w)
    def act(self, fn, r=(), w=()): return self.add("act", fn, r, w)
    def dve(self, fn, r=(), w=()): return self.add("dve", fn, r, w)
    def pool(self, fn, r=(), w=()): return self.add("pool", fn, r, w)
    def dma(self, fn, r=(), w=(), n=1, q="sp"): return self.add(q, fn, r, w, dma=n)
    def barrier(self): return self.add("bar", None)

    def analyze(self):
        last_w = {}; readers = {}
        dcount = {}; dtotal = {}
        last_op = {e: None for e in ENGS}
        for op in self.ops:
            if op.eng == "bar":
                for e in ENGS:
                    lo = last_op[e]
                    if lo is not None and not lo.dma and lo.eng != "sp":
                        lo.has_dep = True
                last_w = {}; readers = {}
                continue
            if op.dma:
                q = op.eng
                k = dcount.get(q, 0) % NDMASEM
                dcount[q] = dcount.get(q, 0) + 1
                op.sem = ("dma", q, k)
                op.prev_target = dtotal.get((q, k), 0)
                op.target = op.prev_target + 16 * op.dma
                dtotal[(q, k)] = op.target
            else:
                op.sem = ("eng", op.eng)
                last_op[op.eng] = op
            deps = []
            for r in op.reads:
                if r in last_w: deps.append(last_w[r])
            for w in op.writes:
                if w in last_w: deps.append(last_w[w])
                deps.extend(readers.get(w, ()))
            op.deps = []
            for d in deps:
                if d is op: continue
                if (not d.dma) and (not op.dma) and d.eng == "pe" and op.eng == "pe":
                    continue
                d.has_dep = True
                op.deps.append(d)
            for r in op.reads:
                readers.setdefault(r, []).append(op)
            for w in op.writes:
                last_w[w] = op
                readers[w] = []
        seq = {e: 0 for e in ENGS}
        for op in self.ops:
            if op.eng != "bar" and not op.dma and op.has_dep:
                seq[op.eng] += 1
                op.seq = seq[op.eng]
        waited = {e: {} for e in ENGS}
        pending = {e: {} for e in ENGS}
        cur_seq = {e: 0 for e in ENGS}
        cur_dma = {}
        for op in self.ops:
            if op.eng == "bar":
                need = {("eng", e): cur_seq[e] for e in ENGS if cur_seq[e] > 0}
                need.update(cur_dma)
                for e in ENGS:
                    for s, v in need.items():
                        pending[e][s] = max(pending[e].get(s, 0), v)
                continue
            need = dict(pending[op.eng]); pending[op.eng] = {}
            if op.dma and op.prev_target > 0:
                need[op.sem] = max(need.get(op.sem, 0), op.prev_target)
            for d in op.deps:
                v = d.target if d.dma else d.seq
                need[d.sem] = max(need.get(d.sem, 0), v)
            w = waited[op.eng]
            op.waits = {}
            for s, v in need.items():
                if w.get(s, 0) < v:
                    op.waits[s] = v
                    w[s] = v
            if op.dma:
                cur_dma[op.sem] = op.target
            elif op.seq is not None:
                cur_seq[op.eng] = op.seq

    def emit(self):
        nc = self.nc
        self.analyze()
        with contextlib.ExitStack() as st:
            sems = {}
            for e in ("pe", "act", "dve", "pool"):
                sems[("eng", e)] = st.enter_context(nc.semaphore("s_" + e))
            for q in ("sp", "pool", "act"):
                for k in range(NDMASEM):
                    sems[("dma", q, k)] = st.enter_context(nc.semaphore(f"d_{q}{k}"))
            block = st.enter_context(nc.Block())
            engmap = {"pe": "tensor", "act": "scalar", "dve": "vector", "pool": "gpsimd", "sp": "sync"}
            ops = self.ops

            def mk(ename):
                def body(eng):
                    for op in ops:
                        if op.eng != ename: continue
                        for s, v in op.waits.items():
                            eng.wait_ge(sems[s], v)
                        ins = op.fn(eng)
                        if ins is None: continue
                        if op.dma:
                            if not isinstance(ins, (list, tuple)): ins = [ins]
                            assert len(ins) == op.dma, (len(ins), op.dma)
                            for i in ins: i.then_inc(sems[op.sem], 16)
                        elif op.has_dep:
                            if isinstance(ins, (list, tuple)): ins = ins[-1]
                            ins.then_inc(sems[op.sem], 1)
                return body
            for ename, attr in engmap.items():
                getattr(block, attr)(mk(ename))


class Buf:
    def __init__(self, ap, name): self.ap = ap; self.n = name


ESZ = {F32: 4, BF16: 2, U32: 4}


class Arena:
    def __init__(self, tens):
        self.t = tens
        self.cnt = 0

    def view(self, name, off, shape, dtype):
        n = int(np.prod(shape[1:]))
        ap = self.t[0:shape[0], off:off + n * ESZ[dtype]].bitcast(dtype)
        if len(shape) > 2:
            names = ["a%d" % i for i in range(len(shape) - 1)]
            pat = "p (" + " ".join(names) + ") -> p " + " ".join(names)
            ap = ap.rearrange(pat, **{nm: int(sz) for nm, sz in zip(names[1:], shape[2:])})
        self.cnt += 1
        return Buf(ap, name)

    @staticmethod
    def nbytes(shape, dtype):
        return int(np.prod(shape[1:])) * ESZ[dtype]


class Bump:
    def __init__(self, arena, base, limit, tag):
        self.a = arena; self.base = base; self.limit = limit; self.off = base; self.tag = tag

    def alloc(self, name, shape, dtype):
        nb = (Arena.nbytes(shape, dtype) + 31) // 32 * 32
        assert self.off + nb <= self.limit, (self.tag, name, self.off, nb, self.limit)
        b = self.a.view(self.tag + "." + name, self.off, shape, dtype)
        self.off += nb
        return b


def host_constants():
    k = np.arange(128)[:, None, None]
    db = np.arange(16)[None, :, None]
    q = np.arange(128)[None, None, :]
    dist = 128 * db + q - k
    mult = ((dist >= 0) & (dist <= 128)).astype(np.float64) + ((dist >= 0) & (dist % 4 == 0) & (dist <= 512)) \
        + ((dist >= 0) & (dist % 16 == 0) & (dist <= 2048))
    lnm = np.where(mult > 0, np.log(np.maximum(mult, 1.0)), NEGBIG).astype(np.float32)
    negd = np.where(mult > 0, -dist, 0).astype(np.float32)
    c_dil = np.stack([negd.reshape(128, 2048), lnm.reshape(128, 2048)], 0).astype(np.float32)
    kk = np.arange(128)[:, None]; qq = np.arange(128)[None, :]
    c_tri = np.where(kk <= qq, 0.0, NEGBIG).astype(np.float32)
    half = 32
    freqs = (np.float32(10000.0) ** (-np.arange(half, dtype=np.float32) / np.float32(half))).astype(np.float32)
    ang = (np.arange(S, dtype=np.float32)[:, None] * freqs[None, :]).astype(np.float32)
    cos = np.cos(ang).astype(np.float32).T; sin = np.sin(ang).astype(np.float32).T
    c_rope = np.stack([np.concatenate([cos, cos], 0), np.concatenate([sin, sin], 0)], 0)
    return {"c_dil": c_dil, "c_tri": c_tri, "c_rope": np.ascontiguousarray(c_rope.astype(np.float32))}


PART_BOUNDS = [0, 4, 16]
SLOPES = [2.0 ** (-8.0 * (i + 1) / 8) for i in range(8)]


def build_program(dbg=(), stop=99):
    nc = bass.Bass("TRN2", target_bir_lowering=False)

    def din(name, shape, dtype=F32):
        return nc.dram_tensor(name, list(shape), dtype, kind="ExternalInput").ap()

    def dscr(name, shape, dtype):
        kind = "ExternalOutput" if name in dbg else "Internal"
        return nc.dram_tensor(name, list(shape), dtype, kind=kind).ap()

    x = din("x", [S, DM])
    ln1_g = din("ln1_g", [1, DM]); w_in = din("w_in", [DM, 4160])
    q_a_norm = din("q_a_norm", [1, 512]); kv_a_norm = din("kv_a_norm", [1, 512])
    w_uq = din("w_uq", [512, 1536]); w_ukv = din("w_ukv", [512, 2048])
    out_norm_dil = din("out_norm_dil", [1, 1024]); out_norm_mla = din("out_norm_mla", [1, 1024])
    w_o = din("w_o", [DM, DM]); ln2_g = din("ln2_g", [1, DM]); peer_wq = din("peer_wq", [DM, DM])
    sub_keys = din("peer_sub_keys", [16, 128, 128])
    peer_u = din("peer_u", [16384, DM]); peer_v = din("peer_v", [16384, DM]); lnf_g = din("lnf_g", [1, DM])
    c_dil = din("c_dil", [2, 128, 2048]); c_tri = din("c_tri", [128, 128]); c_rope = din("c_rope", [2, 64, S])
    out = nc.dram_tensor("out", [S, DM], F32, kind="ExternalOutput").ap()

    qaT_d = dscr("qaT_d", [8, 128, S], BF16); kaT_d = dscr("kaT_d", [8, 128, S], BF16); va_d = dscr("va_d", [8, S, 128], BF16)
    qnT_d = dscr("qnT_d", [8, 128, S], BF16); qpT_d = dscr("qpT_d", [8, 64, S], BF16)
    knT_d = dscr("knT_d", [8, 128, S], BF16); kpT_d = dscr("kpT_d", [64, S], BF16); vm_d = dscr("vm_d", [8, S, 128], BF16)
    h2_d = dscr("h2_d", [S, DM], F32)
    xT_d = dscr("xT_d", [128, 16, S], BF16)
    G_d = dscr("G_d", [16, 128, 128, 128], BF16)
    W_d = dscr("W_d", [128, 128, S], BF16)
    onT_d = dscr("onT_d", [128, 16, S], BF16)
    dbg_d = dscr("dbg_d", [128, 4096], F32)
    sc_d = dscr("sc_d", [16, 128, S], F32)
    UT_d = dscr("UT_d", [128, 128, 16, 128], BF16)

    with contextlib.ExitStack() as st:
        arena_t = st.enter_context(nc.sbuf_tensor("arena", [128, 204 * KB], U8))
        ps_t = st.enter_context(nc.psum_tensor("ps", [128, 4096], F32))
        st.enter_context(nc.allow_non_contiguous_dma("small strided parameter loads"))
        AR = Arena(arena_t)
        P = Prog(nc)

        def bank(i, n=1):
            return ps_t[:, i * 512:(i + n) * 512]

        def bankb(i, n=1):
            return ps_t[:, i * 512:(i + n) * 512].bitcast(BF16)

        def psn(i): return "ps%d" % i

        CONST0, A0, B0, C0, END = 0, 8 * KB, 72 * KB, 136 * KB, 204 * KB
        cb = Bump(AR, CONST0, A0, "K")
        ident_bf = cb.alloc("ident_bf", [128, 128], BF16)
        ident_f = cb.alloc("ident_f", [128, 128], F32)
        ones_bf = cb.alloc("ones_bf", [128, 128], BF16)
        iota_bf = cb.alloc("iota_bf", [128, 128], BF16)
        iota16 = cb.alloc("iota16", [128, 16], F32)
        iota_f = cb.alloc("iota_f", [128, 128], F32)
        mhalf = cb.alloc("mhalf", [128, 1], F32)
        ssA = cb.alloc("ssA", [128, 16], F32); tA = cb.alloc("tA", [128, 16], F32); rsA = cb.alloc("rsA", [128, 16], F32)
        gcol = cb.alloc("gcol", [128, 16], F32)
        keysT = cb.alloc("keysT", [128, 16, 128], BF16)

        P.pool(lambda e: e.memset(ident_bf.ap, 0.0), w=[ident_bf.n])
        P.pool(lambda e: e.affine_select(out=ident_bf.ap, in_=ident_bf.ap, pattern=[[-1, 128]], compare_op=ALU.not_equal,
                                         fill=1.0, base=0, channel_multiplier=1), r=[ident_bf.n], w=[ident_bf.n])
        P.pool(lambda e: e.memset(ident_f.ap, 0.0), w=[ident_f.n])
        P.pool(lambda e: e.affine_select(out=ident_f.ap, in_=ident_f.ap, pattern=[[-1, 128]], compare_op=ALU.not_equal,
                                         fill=1.0, base=0, channel_multiplier=1), r=[ident_f.n], w=[ident_f.n])
        P.pool(lambda e: e.memset(ones_bf.ap, 1.0), w=[ones_bf.n])
        P.pool(lambda e: e.iota(iota_f.ap, pattern=[[1, 128]], base=0, channel_multiplier=0, allow_small_or_imprecise_dtypes=True), w=[iota_f.n])
        P.pool(lambda e: e.iota(iota16.ap, pattern=[[1, 16]], base=0, channel_multiplier=0, allow_small_or_imprecise_dtypes=True), w=[iota16.n])
        P.dve(lambda e: e.tensor_copy(out=iota_bf.ap, in_=iota_f.ap), r=[iota_f.n], w=[iota_bf.n])
        P.dve(lambda e: e.memset(mhalf.ap, -0.5), w=[mhalf.n])

        def rstd_ops(ss_ap, t_ap, rs_ap, names, n_feat):
            P.dve(lambda e: e.tensor_scalar(out=t_ap, in0=ss_ap, scalar1=1.0 / n_feat, scalar2=EPS, op0=ALU.mult, op1=ALU.add),
                  r=[names[0]], w=[names[1]])
            P.pool(lambda e: e.tensor_tensor(out=rs_ap, in0=t_ap, in1=mhalf.ap.to_broadcast(list(t_ap.shape)), op=ALU.pow),
                   r=[names[1], mhalf.n], w=[names[2]])

        def norm_T(tag, src, g_dram, xT):
            cbp = Bump(AR, C0, END, tag)
            xs = [cbp.alloc("xs%d" % i, [128, DM], F32) for i in range(4)]
            xnb = [cbp.alloc("xnb%d" % i, [128, DM], BF16) for i in range(2)]
            sq = cbp.alloc("sq", [128, DM], BF16)
            grep = cbp.alloc("grep", [128, DM], F32)
            P.dma(lambda e: e.dma_start(out=grep.ap, in_=g_dram.broadcast_to([128, DM])), w=[grep.n])
            def st1(c):
                s4 = c % 4
                P.dma(lambda e: e.dma_start(out=xs[s4].ap, in_=src[c * 128:(c + 1) * 128, :]), w=[xs[s4].n])
                nm = [tag + ".ss%d" % c, tag + ".t%d" % c, tag + ".rs%d" % c]
                P.act(lambda e: e.activation(out=sq.ap, in_=xs[s4].ap, func=AF.Square, accum_out=ssA.ap[:, c:c + 1]),
                      r=[xs[s4].n], w=[sq.n, nm[0]])
                rstd_ops(ssA.ap[:, c:c + 1], tA.ap[:, c:c + 1], rsA.ap[:, c:c + 1], nm, DM)

            def st2(c):
                s = c % 2; s4 = c % 4; b0 = 2 * s
                P.dve(lambda e: e.scalar_tensor_tensor(out=xnb[s].ap, in0=xs[s4].ap, scalar=rsA.ap[:, c:c + 1], in1=grep.ap,
                                                       op0=ALU.mult, op1=ALU.mult),
                      r=[xs[s4].n, tag + ".rs%d" % c, grep.n], w=[xnb[s].n])
                P.pe(lambda e: [e.transpose(out=bankb(b0, 2)[:, k * 128:(k + 1) * 128], in_=xnb[s].ap[:, k * 128:(k + 1) * 128],
                                            identity=ident_bf.ap) for k in range(16)],
                     r=[xnb[s].n, ident_bf.n], w=[psn(b0), psn(b0 + 1)])

            def st3(c):
                b0 = 2 * (c % 2)
                P.act(lambda e: e.activation(out=xT.ap[:, :, c * 128:(c + 1) * 128],
                                             in_=bankb(b0, 2).rearrange("p (k t) -> p k t", t=128), func=AF.Copy),
                      r=[], w=[psn(b0), psn(b0 + 1), xT.n + ".c%d" % c])

            for c in range(NT + 2):
                if c < NT: st1(c)
                if 0 <= c - 1 < NT: st2(c - 1)
                if 0 <= c - 2 < NT: st3(c - 2)

        def xT_reads(xT): return [xT.n + ".c%d" % c for c in range(NT)]

        def load_w(dst, wsrc, c0, n, K):
            src = wsrc.rearrange("(k p) c -> p k c", p=128)[:, :, c0:c0 + n]
            P.dma(lambda e: e.dma_start(out=dst.ap[:, 0:K, 0:n], in_=src), w=[dst.n], q="pool")

        evac_rr = [0]

        def evac(out_ap, in_ap, r, w):
            evac_rr[0] ^= 1
            if evac_rr[0]:
                P.act(lambda e: e.activation(out=out_ap, in_=in_ap, func=AF.Copy), r=r, w=w)
            else:
                P.dve(lambda e: e.tensor_copy(out=out_ap, in_=in_ap), r=r, w=w)

        xnT = AR.view("A.xT", A0, [128, 16, S], BF16)
        norm_T("n1", x, ln1_g, xnT)
        P.barrier()
        if stop <= 1:
            P.dma(lambda e: e.dma_start(out=xT_d, in_=xnT.ap), r=[], w=["xT_d"])
            return finish(nc, P, ["xT_d"])

        bb = Bump(AR, B0, C0, "B2"); cc = Bump(AR, C0, END, "C2")
        cqnT = bb.alloc("cqnT", [128, 4, S], BF16); ckvnT = bb.alloc("ckvnT", [128, 4, S], BF16)
        rope_sb = bb.alloc("rope", [64, 2, S], F32)
        wkr = bb.alloc("wkr", [128, 16, 64], BF16); wkrs = bb.alloc("wkrs", [128, 16, 64], BF16)
        gq = [bb.alloc("gq%d" % i, [128, 512], F32) for i in range(2)]
        wb = [cc.alloc("wb%d" % i, [128, 16, 512], BF16) for i in range(2)]
        stg = [cc.alloc("stg%d" % i, [128, S], BF16) for i in range(2)]
        sq2 = cc.alloc("sq", [128, 512], BF16)
        cqn = [cc.alloc("cqn%d" % i, [128, 512], BF16) for i in range(2)]
        rt = [cc.alloc("rt%d" % i, [64, 512], F32) for i in range(3)]
        P.dma(lambda e: e.dma_start(out=rope_sb.ap, in_=c_rope.rearrange("a p t -> p a t")), w=[rope_sb.n])
        P.dma(lambda e: e.dma_start(out=gq[0].ap, in_=q_a_norm.broadcast_to([128, 512])), w=[gq[0].n])
        P.dma(lambda e: e.dma_start(out=gq[1].ap, in_=kv_a_norm.broadcast_to([128, 512])), w=[gq[1].n])
        xr = xT_reads(xnT)
        psrr = [0]

        def next_bank():
            psrr[0] = (psrr[0] + 1) % 4
            return 4 + psrr[0]

        def mm_fm(wt, col0, M, rhsT, K, tg, bk, rnames):
            P.pe(lambda e: [e.matmul(bank(bk)[0:M, :], lhsT=wt.ap[:, k, col0:col0 + M], rhs=rhsT.ap[:, k, tg * 512:(tg + 1) * 512],
                                     start=(k == 0), stop=(k == K - 1)) for k in range(K)],
                 r=rnames, w=[psn(bk)])

        def mm_tm(lhsT_buf, c, wt, rhs_fn, K, bk, rnames):
            P.pe(lambda e: [e.matmul(bank(bk), lhsT=lhsT_buf.ap[:, k, c * 128:(c + 1) * 128], rhs=rhs_fn(k),
                                     start=(k == 0), stop=(k == K - 1)) for k in range(K)],
                 r=rnames, w=[psn(bk)])

        cqbank = {}
        for which, (col0, dstT, g) in enumerate([(3072, cqnT, gq[0]), (3584, ckvnT, gq[1])]):
            w = wb[which]
            load_w(w, w_in, col0, 512, 16)

            def q1a(c, w=w, which=which):
                bk = 4 + (c % 4)
                cqbank[(which, c)] = bk
                mm_tm(xnT, c, w, (lambda k, w=w: w.ap[:, k, :]), 16, bk, [xnT.n + ".c%d" % c, w.n])

            def q1b(c, which=which):
                bk = cqbank[(which, c)]
                nm = ["2.ss%d" % c, "2.t%d" % c, "2.rs%d" % c]
                P.act(lambda e: e.activation(out=sq2.ap, in_=bank(bk), func=AF.Square, accum_out=ssA.ap[:, c:c + 1]),
                      r=[], w=[psn(bk), sq2.n, nm[0]])
                rstd_ops(ssA.ap[:, c:c + 1], tA.ap[:, c:c + 1], rsA.ap[:, c:c + 1], nm, 512)

            def q2(c, g=g, which=which):
                bk = cqbank[(which, c)]
                s = c % 2
                P.dve(lambda e: e.scalar_tensor_tensor(out=cqn[s].ap, in0=bank(bk), scalar=rsA.ap[:, c:c + 1], in1=g.ap,
                                                       op0=ALU.mult, op1=ALU.mult),
                      r=["2.rs%d" % c, g.n], w=[psn(bk), cqn[s].n])
                P.pe(lambda e: [e.transpose(out=bankb(s)[:, k * 128:(k + 1) * 128], in_=cqn[s].ap[:, k * 128:(k + 1) * 128],
                                            identity=ident_bf.ap) for k in range(4)],
                     r=[cqn[s].n, ident_bf.n], w=[psn(s)])

            def q3(c, dstT=dstT):
                tb = c % 2
                evac(dstT.ap[:, :, c * 128:(c + 1) * 128], bankb(tb)[:, 0:512].rearrange("p (k t) -> p k t", t=128),
                     [], [psn(tb), dstT.n + ".c%d" % c])

            for c in range(NT + 2):
                if c < NT: q1a(c)
                if 0 <= c - 1 < NT: q2(c - 1)
                if c < NT: q1b(c)
                if 0 <= c - 2 < NT: q3(c - 2)
        P.dma(lambda e: e.dma_start(out=wkr.ap, in_=w_in.rearrange("(k p) c -> p k c", p=128)[:, :, 4096:4160]), w=[wkr.n], q="pool")
        P.dve(lambda e: e.tensor_scalar(out=wkrs.ap[:, :, 0:32], in0=wkr.ap[:, :, 32:64], scalar1=-1.0, scalar2=None, op0=ALU.mult),
              r=[wkr.n], w=[wkrs.n + ".a"])
        P.dve(lambda e: e.tensor_copy(out=wkrs.ap[:, :, 32:64], in_=wkr.ap[:, :, 0:32]), r=[wkr.n], w=[wkrs.n + ".b"])

        def rope_fm(wA, wAn, wB, wBn, colA, colB, rhsT, K, rnames, dst_stage):
            rt0, rt1 = rt[0], rt[1]
            for tg in range(4):
                bA = next_bank()
                mm_fm(wA, colA, 64, rhsT, K, tg, bA, rnames + wAn)
                bB = next_bank()
                mm_fm(wB, colB, 64, rhsT, K, tg, bB, rnames + wBn)
                P.dve(lambda e, bA=bA, tg=tg, rt0=rt0: e.tensor_tensor(out=rt0.ap, in0=bank(bA)[0:64, :], in1=rope_sb.ap[:, 0, tg * 512:(tg + 1) * 512], op=ALU.mult),
                      r=[rope_sb.n], w=[psn(bA), rt0.n])
                P.dve(lambda e, bB=bB, tg=tg, rt1=rt1: e.tensor_tensor(out=rt1.ap, in0=bank(bB)[0:64, :], in1=rope_sb.ap[:, 1, tg * 512:(tg + 1) * 512], op=ALU.mult),
                      r=[rope_sb.n], w=[psn(bB), rt1.n])
                P.dve(lambda e, tg=tg, rt0=rt0, rt1=rt1: e.tensor_tensor(out=dst_stage.ap[0:64, tg * 512:(tg + 1) * 512], in0=rt0.ap, in1=rt1.ap, op=ALU.add),
                      r=[rt0.n, rt1.n], w=[dst_stage.n])

        rope_fm(wkr, [wkr.n], wkrs, [wkrs.n + ".a", wkrs.n + ".b"], 0, 0, xnT, 16, xr, stg[0])
        P.dma(lambda e: e.dma_start(out=kpT_d, in_=stg[0].ap[0:64, :]), r=[stg[0].n], w=["kpT_d"], q="act")
        sgi = [0]
        wbi = [0]
        for (col0, dst) in [(0, qaT_d), (1024, kaT_d)]:
            for half in range(2):
                w = wb[wbi[0] % 2]; wbi[0] += 1
                load_w(w, w_in, col0 + half * 512, 512, 16)
                for m in range(4):
                    h = half * 4 + m
                    sg = stg[sgi[0] % 2]; sgi[0] += 1
                    for tg in range(4):
                        bk = next_bank()
                        mm_fm(w, m * 128, 128, xnT, 16, tg, bk, xr + [w.n])
                        evac(sg.ap[:, tg * 512:(tg + 1) * 512], bank(bk), [], [psn(bk), sg.n])
                    P.dma(lambda e, sg=sg, dst=dst, h=h: e.dma_start(out=dst[h], in_=sg.ap), r=[sg.n], w=["qk_d"], q="act")
        for half in range(2):
            w = wb[wbi[0] % 2]; wbi[0] += 1
            load_w(w, w_in, 2048 + half * 512, 512, 16)
            for c in range(NT):
                bk = next_bank()
                mm_tm(xnT, c, w, (lambda k, w=w: w.ap[:, k, :]), 16, bk, [xnT.n + ".c%d" % c, w.n])
                sg = stg[sgi[0] % 2]; sgi[0] += 1
                evac(sg.ap[:, 0:512], bank(bk), [], [psn(bk), sg.n])
                P.dma(lambda e, sg=sg, c=c, half=half: e.dma_start(
                    out=va_d[half * 4:(half + 1) * 4, c * 128:(c + 1) * 128, :].rearrange("h t e -> t h e"),
                    in_=sg.ap[:, 0:512].rearrange("p (h e) -> p h e", e=128)), r=[sg.n], w=["va_d"], q="act")
        P.barrier()
        cc = Bump(AR, C0, END, "C2b")
        wuq = cc.alloc("wuq", [128, 4, 1536], BF16); wuqs = cc.alloc("wuqs", [128, 4, 8, 64], BF16)
        wukv = cc.alloc("wukv", [128, 4, 2048], BF16)
        stg = [cc.alloc("stg%d" % i, [128, S], BF16) for i in range(2)]
        rt = [cc.alloc("rt%d" % i, [64, 512], F32) for i in range(2)]
        load_w(wuq, w_uq, 0, 1536, 4)
        load_w(wukv, w_ukv, 0, 2048, 4)
        wuq_v = wuq.ap.rearrange("p k (h c) -> p k h c", c=192)
        for k in range(4):
            P.dve(lambda e, k=k: e.tensor_scalar(out=wuqs.ap[:, k, :, 0:32], in0=wuq_v[:, k, :, 160:192], scalar1=-1.0, scalar2=None, op0=ALU.mult),
                  r=[wuq.n], w=[wuqs.n + ".a%d" % k])
            P.dve(lambda e, k=k: e.tensor_copy(out=wuqs.ap[:, k, :, 32:64], in_=wuq_v[:, k, :, 128:160]), r=[wuq.n], w=[wuqs.n + ".b%d" % k])
        wuqs_n = [wuqs.n + ".a%d" % k for k in range(4)] + [wuqs.n + ".b%d" % k for k in range(4)]
        wuqs_flat = Buf(wuqs.ap.rearrange("p k h c -> p k (h c)"), wuqs.n)
        cqr = xT_reads(cqnT); ckr = xT_reads(ckvnT)
        sgi = [0]
        for h in range(8):
            sg = stg[sgi[0] % 2]; sgi[0] += 1
            for tg in range(4):
                bk = next_bank()
                mm_fm(wuq, h * 192, 128, cqnT, 4, tg, bk, cqr + [wuq.n])
                evac(sg.ap[:, tg * 512:(tg + 1) * 512], bank(bk), [], [psn(bk), sg.n])
            P.dma(lambda e, sg=sg, h=h: e.dma_start(out=qnT_d[h], in_=sg.ap), r=[sg.n], w=["qnT_d"], q="act")
            sg = stg[sgi[0] % 2]; sgi[0] += 1
            rope_fm(wuq, [wuq.n], wuqs_flat, wuqs_n, h * 192 + 128, h * 64, cqnT, 4, cqr, sg)
            P.dma(lambda e, sg=sg, h=h: e.dma_start(out=qpT_d[h], in_=sg.ap[0:64, :]), r=[sg.n], w=["qpT_d"], q="act")
            sg = stg[sgi[0] % 2]; sgi[0] += 1
            for tg in range(4):
                bk = next_bank()
                mm_fm(wukv, h * 256, 128, ckvnT, 4, tg, bk, ckr + [wukv.n])
                evac(sg.ap[:, tg * 512:(tg + 1) * 512], bank(bk), [], [psn(bk), sg.n])
            P.dma(lambda e, sg=sg, h=h: e.dma_start(out=knT_d[h], in_=sg.ap), r=[sg.n], w=["knT_d"], q="act")
        wukv_v = wukv.ap.rearrange("p k (h c) -> p k h c", c=256)
        for half in range(2):
            for c in range(NT):
                bk = next_bank()
                mm_tm(ckvnT, c, wukv, (lambda k, half=half: wukv_v[:, k, half * 4:(half + 1) * 4, 128:256]), 4, bk,
                      [ckvnT.n + ".c%d" % c, wukv.n])
                sg = stg[sgi[0] % 2]; sgi[0] += 1
                evac(sg.ap[:, 0:512], bank(bk), [], [psn(bk), sg.n])
                P.dma(lambda e, sg=sg, c=c, half=half: e.dma_start(
                    out=vm_d[half * 4:(half + 1) * 4, c * 128:(c + 1) * 128, :].rearrange("h t e -> t h e"),
                    in_=sg.ap[:, 0:512].rearrange("p (h e) -> p h e", e=128)), r=[sg.n], w=["vm_d"], q="act")
        P.barrier()
        if stop <= 2:
            return finish(nc, P, [])

        onT = AR.view("A.onT", A0, [128, 8, S], F32)
        onrm = [AR.view("B.onrm%d" % i, B0 + i * 32 * KB, [128, 8, S], BF16) for i in range(2)]
        tb_ = Bump(AR, B0 + 32 * KB, C0, "B3t")
        negd = tb_.alloc("negd", [128, S], F32); lnm = tb_.alloc("lnm", [128, S], F32)
        cc = Bump(AR, C0, END, "C3")
        qT = [cc.alloc("qT%d" % i, [128, S], BF16) for i in range(2)]
        kT = [cc.alloc("kT%d" % i, [128, S], BF16) for i in range(2)]
        vv = [cc.alloc("v%d" % i, [128, 16, 128], BF16) for i in range(2)]
        qpT = [cc.alloc("qpT%d" % i, [64, S], BF16) for i in range(2)]
        kpT = cc.alloc("kpT", [64, S], BF16)
        biasb = [cc.alloc("bias%d" % i, [128, S], F32) for i in range(2)]
        tri = cc.alloc("tri", [128, 128], F32)
        sp_ = [cc.alloc("sp%d" % i, [128, 512], F32) for i in range(2)]
        pT = [cc.alloc("pT%d" % i, [128, 512], BF16) for i in range(3)]
        rl = cc.alloc("rl", [128, 512], F32)
        sq3 = [cc.alloc("sq%d" % i, [128, 512], BF16) for i in range(2)]
        rsb = cc.alloc("rsb", [128, 512], F32); tt3 = cc.alloc("tt3", [128, 512], F32)
        epsb = cb.alloc("epsb", [128, 1], F32)
        P.dve(lambda e: e.memset(epsb.ap, EPS), w=[epsb.n])
        P.dma(lambda e: e.dma_start(out=negd.ap, in_=c_dil[0]), w=[negd.n])
        P.dma(lambda e: e.dma_start(out=lnm.ap, in_=c_dil[1]), w=[lnm.n])
        P.dma(lambda e: e.dma_start(out=tri.ap, in_=c_tri), w=[tri.n])
        P.dma(lambda e: e.dma_start(out=kpT.ap, in_=kpT_d), w=[kpT.n])

        cnt = {"sp": 0, "pT": 0}
        for grp in range(2):
            scale = (128 ** -0.5) if grp == 0 else (192 ** -0.5)
            gdram = out_norm_dil if grp == 0 else out_norm_mla
            P.dma(lambda e, gdram=gdram, grp=grp: e.dma_start(out=gcol.ap[:, grp * 8:(grp + 1) * 8], in_=gdram.rearrange("o (h p) -> p (o h)", p=128)),
                  w=[gcol.n + "%d" % grp])

            def head_loads(h, grp=grp):
                hb = h % 2
                if grp == 0:
                    P.dma(lambda e: e.dma_start(out=qT[hb].ap, in_=qaT_d[h]), w=[qT[hb].n])
                    P.dma(lambda e: e.dma_start(out=kT[hb].ap, in_=kaT_d[h]), w=[kT[hb].n])
                    P.dma(lambda e: e.dma_start(out=vv[hb].ap, in_=va_d[h].rearrange("(kb p) e -> p kb e", p=128)), w=[vv[hb].n])
                    P.dve(lambda e: e.scalar_tensor_tensor(out=biasb[hb].ap, in0=negd.ap, scalar=float(SLOPES[h]), in1=lnm.ap,
                                                           op0=ALU.mult, op1=ALU.add), r=[negd.n, lnm.n], w=[biasb[hb].n])
                else:
                    P.dma(lambda e: e.dma_start(out=qT[hb].ap, in_=qnT_d[h]), w=[qT[hb].n])
                    P.dma(lambda e: e.dma_start(out=kT[hb].ap, in_=knT_d[h]), w=[kT[hb].n])
                    P.dma(lambda e: e.dma_start(out=vv[hb].ap, in_=vm_d[h].rearrange("(kb p) e -> p kb e", p=128)), w=[vv[hb].n])
                    P.dma(lambda e: e.dma_start(out=qpT[hb].ap, in_=qpT_d[h]), w=[qpT[hb].n])

            steps = []
            for h in range(8):
                for qg in range(4):
                    nkb = 4 * qg + 4
                    for kb in range(nkb):
                        steps.append((h, qg, kb, nkb))

            def geom(st_):
                h, qg, kb, nkb = st_
                q0 = max(qg * 512, kb * 128)
                n = (qg + 1) * 512 - q0
                return h, qg, kb, nkb, q0, n, q0 - qg * 512

            def emit_qk(si, grp=grp):
                h, qg, kb, nkb, q0, n, c0 = geom(steps[si])
                hb = h % 2; sb = SBK[si % 4]
                rn = [qT[hb].n, kT[hb].n] + ([qpT[hb].n, kpT.n] if grp else [])

                def f_qk(e):
                    i = e.matmul(bank(sb)[:, 0:n], lhsT=kT[hb].ap[:, kb * 128:(kb + 1) * 128], rhs=qT[hb].ap[:, q0:q0 + n],
                                 start=True, stop=(grp == 0))
                    if grp:
                        i = e.matmul(bank(sb)[:, 0:n], lhsT=kpT.ap[:, kb * 128:(kb + 1) * 128], rhs=qpT[hb].ap[:, q0:q0 + n],
                                     start=False, stop=True)
                    return i
                P.pe(f_qk, r=rn, w=[psn(sb)])

            def emit_sm(si, grp=grp, scale=scale):
                h, qg, kb, nkb, q0, n, c0 = geom(steps[si])
                hb = h % 2; sb = SBK[si % 4]
                pt = pT[cnt["pT"] % 3]; cnt["pT"] += 1
                diag = kb >= 4 * qg
                if grp == 0:
                    spb = sp_[cnt["sp"] % 2]; cnt["sp"] += 1
                    db0 = q0 // 128 - kb
                    P.dve(lambda e: e.scalar_tensor_tensor(
                        out=spb.ap[:, 0:n], in0=bank(sb)[:, 0:n], scalar=float(scale), in1=biasb[hb].ap[:, db0 * 128:db0 * 128 + n],
                        op0=ALU.mult, op1=ALU.add), r=[biasb[hb].n], w=[psn(sb), spb.n])
                    P.act(lambda e: e.activation(out=pt.ap[:, 0:n], in_=spb.ap[:, 0:n], func=AF.Exp), r=[spb.n], w=[pt.n])
                else:
                    if diag:
                        spb = sp_[cnt["sp"] % 2]; cnt["sp"] += 1
                        P.dve(lambda e: e.scalar_tensor_tensor(
                            out=spb.ap[:, 0:128], in0=bank(sb)[:, 0:128], scalar=float(scale), in1=tri.ap,
                            op0=ALU.mult, op1=ALU.add), r=[tri.n], w=[psn(sb), spb.n])
                        P.act(lambda e: e.activation(out=pt.ap[:, 0:128], in_=spb.ap[:, 0:128], func=AF.Exp), r=[spb.n], w=[pt.n + ".a"])
                        if n > 128:
                            P.act(lambda e: e.activation(out=pt.ap[:, 128:n], in_=bank(sb)[:, 128:n], func=AF.Exp, scale=float(scale)),
                                  r=[], w=[psn(sb), pt.n + ".b"])
                    else:
                        P.act(lambda e: e.activation(out=pt.ap[:, 0:n], in_=bank(sb)[:, 0:n], func=AF.Exp, scale=float(scale)),
                              r=[], w=[psn(sb), pt.n + ".a", pt.n + ".b"])
                return pt

            def emit_pv(si, pt):
                h, qg, kb, nkb, q0, n, c0 = geom(steps[si])
                hb = h % 2
                ob = 2 + 2 * (qg % 2)
                first = (kb == 0); last = (kb == nkb - 1)

                def f_pv(e):
                    e.matmul(bank(ob)[:, c0:c0 + n], lhsT=vv[hb].ap[:, kb, :], rhs=pt.ap[:, 0:n], start=first, stop=last, skip_group_check=True)
                    return e.matmul(bank(ob + 1)[:, c0:c0 + n], lhsT=ones_bf.ap, rhs=pt.ap[:, 0:n], start=first, stop=last, skip_group_check=True)
                P.pe(f_pv, r=[pt.n, pt.n + ".a", pt.n + ".b", vv[hb].n, ones_bf.n], w=[psn(ob), psn(ob + 1)])
                if last:
                    P.act(lambda e: e.activation(out=tt3.ap, in_=bank(ob + 1), func=AF.Ln), r=[], w=[psn(ob + 1), tt3.n])
                    P.act(lambda e: e.activation(out=rl.ap, in_=tt3.ap, func=AF.Exp, scale=-1.0), r=[tt3.n], w=[rl.n])
                    P.dve(lambda e: e.tensor_tensor(out=onT.ap[:, h, qg * 512:(qg + 1) * 512], in0=bank(ob), in1=rl.ap, op=ALU.mult),
                          r=[rl.n], w=[psn(ob), onT.n + ".%d.%d" % (h, qg)])

            SBK = [0, 1, 6, 7]
            LA = 3
            head_loads(0)
            for j in range(LA):
                emit_qk(j)
            for si in range(len(steps)):
                h, qg, kb, nkb = steps[si]
                if qg == 0 and kb == 0 and h + 1 < 8:
                    head_loads(h + 1)
                pt = emit_sm(si)
                if si + LA < len(steps):
                    emit_qk(si + LA)
                emit_pv(si, pt)
            if grp == 1:
                P.barrier()
            for qg in range(4):
                for h in range(8):
                    s = h % 2
                    P.act(lambda e, h=h, qg=qg, s=s: e.activation(out=sq3[s].ap, in_=onT.ap[:, h, qg * 512:(qg + 1) * 512], func=AF.Square),
                          r=[onT.n + ".%d.%d" % (h, qg)], w=[sq3[s].n])
                    P.pe(lambda e, h=h, s=s: e.matmul(bank(0), lhsT=ones_bf.ap, rhs=sq3[s].ap, start=(h == 0), stop=(h == 7)),
                         r=[sq3[s].n, ones_bf.n], w=[psn(0)])
                P.act(lambda e: e.activation(out=tt3.ap, in_=bank(0), func=AF.Sqrt, scale=1.0 / 1024, bias=epsb.ap), r=[epsb.n], w=[psn(0), tt3.n])
                P.dve(lambda e: e.reciprocal(out=rsb.ap, in_=tt3.ap), r=[tt3.n], w=[rsb.n])
                for h in range(8):
                    P.dve(lambda e, h=h, qg=qg, grp=grp: e.scalar_tensor_tensor(
                        out=onrm[grp].ap[:, h, qg * 512:(qg + 1) * 512], in0=onT.ap[:, h, qg * 512:(qg + 1) * 512],
                        scalar=gcol.ap[:, grp * 8 + h:grp * 8 + h + 1], in1=rsb.ap, op0=ALU.mult, op1=ALU.mult),
                        r=[onT.n + ".%d.%d" % (h, qg), rsb.n, gcol.n + "%d" % grp], w=[onrm[grp].n + ".%d" % qg])
            if grp == 1:
                P.barrier()
        if stop <= 3:
            P.dma(lambda e: e.dma_start(out=onT_d[:, 0:8, :], in_=onrm[0].ap), r=[], w=["onT_d"])
            P.dma(lambda e: e.dma_start(out=onT_d[:, 8:16, :], in_=onrm[1].ap), r=[], w=["onT_d"])
            return finish(nc, P, ["onT_d"])

        cc = Bump(AR, C0, END, "C4")
        wo = [cc.alloc("wo%d" % i, [128, 16, 512], BF16) for i in range(2)]
        xt = [cc.alloc("xt%d" % i, [128, 512], F32) for i in range(2)]
        ht = [cc.alloc("ht%d" % i, [128, 512], F32) for i in range(2)]
        onrm_all = [onrm[0].n + ".%d" % q for q in range(4)] + [onrm[1].n + ".%d" % q for q in range(4)]
        for dg in range(4):
            w = wo[dg % 2]
            load_w(w, w_o, dg * 512, 512, 16)
            for c in range(NT):
                s = c % 2
                P.dma(lambda e, c=c, dg=dg, s=s: e.dma_start(out=xt[s].ap, in_=x[c * 128:(c + 1) * 128, dg * 512:(dg + 1) * 512]), w=[xt[s].n])
                bk = next_bank()
                P.pe(lambda e, c=c, w=w, bk=bk: [e.matmul(bank(bk), lhsT=onrm[k // 8].ap[:, k % 8, c * 128:(c + 1) * 128], rhs=w.ap[:, k, :],
                                                          start=(k == 0), stop=(k == 15)) for k in range(16)],
                     r=onrm_all + [w.n], w=[psn(bk)])
                P.dve(lambda e, bk=bk, s=s: e.tensor_tensor(out=ht[s].ap, in0=bank(bk), in1=xt[s].ap, op=ALU.add),
                      r=[xt[s].n], w=[psn(bk), ht[s].n])
                P.dma(lambda e, c=c, dg=dg, s=s: e.dma_start(out=h2_d[c * 128:(c + 1) * 128, dg * 512:(dg + 1) * 512], in_=ht[s].ap),
                      r=[ht[s].n], w=["h2_d"], q="act")
        P.barrier()
        if stop <= 4:
            return finish(nc, P, [])
        xn2T = AR.view("A.xT", A0, [128, 16, S], BF16)
        norm_T("n2", h2_d, ln2_g, xn2T)
        P.barrier()
        P.dma(lambda e: e.dma_start(out=xT_d, in_=xn2T.ap), r=[], w=["xT_d"])

        qp = AR.view("B.qp", B0, [128, 16, S], BF16)
        cc = Bump(AR, C0, END, "C5a")
        wq = [cc.alloc("wq%d" % i, [128, 16, 512], BF16) for i in range(2)]
        kn = cc.alloc("kn", [128, 16, 128], BF16)
        P.dma(lambda e: e.dma_start(out=kn.ap, in_=sub_keys.rearrange("g n c -> n g c")), w=[kn.n], q="pool")
        P.pe(lambda e: [e.transpose(out=bankb(0, 2)[:, g * 128:(g + 1) * 128], in_=kn.ap[:, g, :], identity=ident_bf.ap) for g in range(16)],
             r=[kn.n, ident_bf.n], w=[psn(0), psn(1)])
        evac(keysT.ap, bankb(0, 2).rearrange("p (g n) -> p g n", n=128), [], [psn(0), psn(1), keysT.n])
        x2r = xT_reads(xn2T)
        for blk in range(4):
            w = wq[blk % 2]
            load_w(w, peer_wq, blk * 512, 512, 16)
            for m in range(4):
                g = blk * 4 + m
                for tg in range(4):
                    bk = next_bank()
                    mm_fm(w, m * 128, 128, xn2T, 16, tg, bk, x2r + [w.n])
                    evac(qp.ap[:, g, tg * 512:(tg + 1) * 512], bank(bk), [], [psn(bk), qp.n + ".%d" % g])
        cc5 = Bump(AR, C0 + 40 * KB, END, "C5s")
        ssc = [cc5.alloc("ssc%d" % i, [128, S], F32) for i in range(2)]
        for c in range(NT):
            pb = 4 * (c % 2)
            for g in range(16):
                P.pe(lambda e, g=g, c=c, pb=pb: e.matmul(bank(pb + g // 4)[:, (g % 4) * 128:(g % 4 + 1) * 128], lhsT=qp.ap[:, g, c * 128:(c + 1) * 128],
                                                         rhs=keysT.ap[:, g, :], start=True, stop=True, skip_group_check=True),
                     r=[qp.n + ".%d" % g, keysT.n], w=[psn(pb + g // 4)])
            for b4 in range(4):
                evac(ssc[c % 2].ap[:, b4 * 512:(b4 + 1) * 512], bank(pb + b4), [], [psn(pb + b4), ssc[c % 2].n])
            P.dma(lambda e, c=c: e.dma_start(out=sc_d[c], in_=ssc[c % 2].ap), r=[ssc[c % 2].n], w=["sc_d"], q="act")
        P.barrier()
        if stop <= 5:
            return finish(nc, P, ["xT_d"])

        PB = PART_BOUNDS
        NP = len(PB) - 1
        def part_of(c): return max(p for p in range(NP) if PB[p] <= c)
        def nch(p): return PB[p + 1] - PB[p]
        def tok0(p): return PB[p] * 128
        def ntok(p): return nch(p) * 128
        CPPo = max([nch(p) for p in range(NP - 1)] + [4])
        TPo = CPPo * 128
        ab = Bump(AR, A0, END, "X5")
        ssb2 = [ab.alloc("ssb%d" % i, [128, 16, 128], F32) for i in range(2)]
        tv = ab.alloc("tv", [128, 16, 16], F32); ti = ab.alloc("ti", [128, 16, 16], U32); tif = ab.alloc("tif", [128, 16, 16], F32)
        cand = ab.alloc("cand", [128, 8, 256], F32)
        cv = ab.alloc("cv", [128, 8, 16], F32); ci = ab.alloc("ci", [128, 8, 16], U32)
        aku = ab.alloc("aku", [128, 8, 16], U32); bku = ab.alloc("bku", [128, 8, 16], U32)
        akf = ab.alloc("akf", [128, 8, 16], F32); bkf = ab.alloc("bkf", [128, 8, 16], F32)
        oh = [ab.alloc("oh%d" % i, [128, 8, 16, 16], F32) for i in range(2)]
        ik = ab.alloc("ik", [128, 128], F32); jk = ab.alloc("jk", [128, 128], F32); gk = ab.alloc("gk", [128, 128], F32)
        dd = ab.alloc("dd", [128, 8, 16], F32); ee = ab.alloc("ee", [128, 8, 16], F32); zz = ab.alloc("zz", [128, 8], F32); rz = ab.alloc("rz", [128, 8], F32)
        tT = ab.alloc("tT", [128, 3, 128], F32)
        PQ = [(ab.alloc("P%d" % i, [128, 16, 128], BF16), ab.alloc("Q%d" % i, [128, 16, 128], BF16)) for i in range(3)]
        Gc = ab.alloc("Gc", [128, 128, 128], BF16)
        xh = ab.alloc("xh", [128, 16, TPo], BF16)
        Gg = [ab.alloc("Gg%d" % i, [128, CPPo, 4, 128], BF16) for i in range(2)]
        Un = [ab.alloc("Un%d" % i, [128, 2, DM], BF16) for i in range(3)]
        UT = [ab.alloc("UT%d" % i, [128, 16, 128], BF16) for i in range(2)]
        Wt = [ab.alloc("Wt%d" % i, [128, TPo], BF16) for i in range(2)]
        ga = [ab.alloc("ga%d" % i, [128, 512], BF16) for i in range(2)]
        tvv = tv.ap.rearrange("p (h s) k -> p h s k", s=2)
        tifv = tif.ap.rearrange("p (h s) k -> p h s k", s=2)
        candv = cand.ap.rearrange("p h (a b) -> p h a b", b=16)
        iota16v = iota16.ap.unsqueeze(1).unsqueeze(1).broadcast_to([128, 8, 16, 16])
        pqi = [0]
        gbi = [0]

        def stage_A(c, pieces=None):
            sb_ = ssb2[c % 2]
            if pieces is None or 0 in pieces:
                P.dma(lambda e: e.dma_start(out=sb_.ap.rearrange("p g n -> p (g n)"), in_=sc_d[c]), r=["sc_d"], w=[sb_.n + ".ld"])
            for g in range(16):
                if pieces is not None and (g // 2) not in pieces:
                    continue
                tn = tv.n + ".%d" % g
                sn = sb_.n + ".g%d" % g
                P.dve(lambda e, g=g: e.max(out=tv.ap[:, g, 0:8], in_=sb_.ap[:, g, :]), r=[sb_.n + ".ld", sn], w=[tn + "a"])
                P.dve(lambda e, g=g: e.max_index(out=ti.ap[:, g, 0:8], in_max=tv.ap[:, g, 0:8], in_values=sb_.ap[:, g, :]), r=[sb_.n + ".ld", sn, tn + "a"], w=[tn + "ia"])
                P.dve(lambda e, g=g: e.match_replace(out=sb_.ap[:, g, :], in_to_replace=tv.ap[:, g, 0:8], in_values=sb_.ap[:, g, :], imm_value=-1e30),
                      r=[sb_.n + ".ld", tn + "a"], w=[sn])
                P.dve(lambda e, g=g: e.max(out=tv.ap[:, g, 8:16], in_=sb_.ap[:, g, :]), r=[sb_.n + ".ld", sn], w=[tn + "b"])
                P.dve(lambda e, g=g: e.max_index(out=ti.ap[:, g, 8:16], in_max=tv.ap[:, g, 8:16], in_values=sb_.ap[:, g, :]), r=[sb_.n + ".ld", sn, tn + "b"], w=[tn + "ib"])

        def stage_B(c):
            tvn = [tv.n + ".%d%s" % (g, s_) for g in range(16) for s_ in "ab"]
            tin = [tv.n + ".%d%s" % (g, s_) for g in range(16) for s_ in ("ia", "ib")]
            P.dve(lambda e: e.tensor_copy(out=tif.ap, in_=ti.ap), r=tin, w=[tif.n])
            P.dve(lambda e: e.tensor_tensor(out=candv, in0=tvv[:, :, 0, :].unsqueeze(3).broadcast_to([128, 8, 16, 16]),
                                            in1=tvv[:, :, 1, :].unsqueeze(2).broadcast_to([128, 8, 16, 16]), op=ALU.add),
                  r=tvn, w=[cand.n])
            for h in range(8):
                cn = cv.n + ".%d" % h
                P.dve(lambda e, h=h: e.max(out=cv.ap[:, h, 0:8], in_=cand.ap[:, h, :]), r=[cand.n], w=[cn + "a"])
                P.dve(lambda e, h=h: e.max_index(out=ci.ap[:, h, 0:8], in_max=cv.ap[:, h, 0:8], in_values=cand.ap[:, h, :]), r=[cand.n, cn + "a"], w=[cn + "ia"])
                P.dve(lambda e, h=h: e.match_replace(out=cand.ap[:, h, :], in_to_replace=cv.ap[:, h, 0:8], in_values=cand.ap[:, h, :], imm_value=-1e30),
                      r=[cn + "a"], w=[cand.n])
                P.dve(lambda e, h=h: e.max(out=cv.ap[:, h, 8:16], in_=cand.ap[:, h, :]), r=[cand.n], w=[cn + "b"])
                P.dve(lambda e, h=h: e.max_index(out=ci.ap[:, h, 8:16], in_max=cv.ap[:, h, 8:16], in_values=cand.ap[:, h, :]), r=[cand.n, cn + "b"], w=[cn + "ib"])
            cvn = [cv.n + ".%d%s" % (h, s_) for h in range(8) for s_ in "ab"]
            cin = [cv.n + ".%d%s" % (h, s_) for h in range(8) for s_ in ("ia", "ib")]
            P.dve(lambda e: e.tensor_tensor(out=dd.ap, in0=cv.ap, in1=cv.ap[:, :, 0:1].broadcast_to([128, 8, 16]), op=ALU.subtract), r=cvn, w=[dd.n])
            P.act(lambda e: e.activation(out=ee.ap, in_=dd.ap, func=AF.Exp), r=[dd.n], w=[ee.n])
            P.dve(lambda e: e.tensor_single_scalar(out=aku.ap, in_=ci.ap, scalar=4, op=ALU.logical_shift_right), r=cin, w=[aku.n])
            P.dve(lambda e: e.tensor_single_scalar(out=bku.ap, in_=ci.ap, scalar=15, op=ALU.bitwise_and), r=cin, w=[bku.n])
            P.dve(lambda e: e.tensor_copy(out=akf.ap, in_=aku.ap), r=[aku.n], w=[akf.n])
            P.dve(lambda e: e.tensor_copy(out=bkf.ap, in_=bku.ap), r=[bku.n], w=[bkf.n])
            for side, (sel, dst) in enumerate([(akf, ik), (bkf, jk)]):
                o0, o1 = oh
                P.dve(lambda e, sel=sel, o0=o0: e.tensor_tensor(out=o0.ap, in0=sel.ap.unsqueeze(3).broadcast_to([128, 8, 16, 16]), in1=iota16v, op=ALU.is_equal),
                      r=[sel.n, iota16.n], w=[o0.n])
                P.dve(lambda e, side=side, o0=o0, o1=o1: e.tensor_tensor(out=o1.ap, in0=o0.ap, in1=tifv[:, :, side, :].unsqueeze(2).broadcast_to([128, 8, 16, 16]), op=ALU.mult),
                      r=[o0.n, tif.n], w=[o1.n])
                P.dve(lambda e, dst=dst, o1=o1: e.tensor_reduce(out=dst.ap.rearrange("p (h k) -> p h k", k=16), in_=o1.ap, axis=AX.X, op=ALU.add),
                      r=[o1.n], w=[dst.n])
            P.dve(lambda e: e.tensor_reduce(out=zz.ap, in_=ee.ap, axis=AX.X, op=ALU.add), r=[ee.n], w=[zz.n])
            P.dve(lambda e: e.reciprocal(out=rz.ap, in_=zz.ap), r=[zz.n], w=[rz.n])
            P.dve(lambda e: e.tensor_tensor(out=gk.ap.rearrange("p (h k) -> p h k", k=16), in0=ee.ap, in1=rz.ap.unsqueeze(2).broadcast_to([128, 8, 16]), op=ALU.mult),
                  r=[ee.n, rz.n], w=[gk.n])

        def stage_Bpe(c):
            P.pe(lambda e: [e.transpose(out=bank(0)[:, i * 128:(i + 1) * 128], in_=src.ap, identity=ident_f.ap) for i, src in enumerate([ik, jk, gk])],
                 r=[ik.n, jk.n, gk.n, ident_f.n], w=[psn(0)])
            P.act(lambda e: e.activation(out=tT.ap, in_=bank(0)[:, 0:384].rearrange("p (a t) -> p a t", t=128), func=AF.Copy), r=[], w=[psn(0), tT.n])

        def stage_C_group(c, t0):
            Pb, Qb = PQ[pqi[0] % len(PQ)]; pqi[0] += 1

            def f_q(e):
                return e.tensor_tensor(out=Qb.ap, in0=iota_bf.ap.unsqueeze(1).broadcast_to([128, 16, 128]),
                                       in1=tT.ap[:, 1, t0:t0 + 16].unsqueeze(2).broadcast_to([128, 16, 128]), op=ALU.is_equal)

            def f_p(e):
                return e.tensor_tensor(out=Pb.ap, in0=iota_bf.ap.unsqueeze(1).broadcast_to([128, 16, 128]),
                                       in1=tT.ap[:, 0, t0:t0 + 16].unsqueeze(2).broadcast_to([128, 16, 128]), op=ALU.is_equal)

            def f_pg(e):
                for tt in range(16):
                    i = e.activation(out=Pb.ap[:, tt, :], in_=Pb.ap[:, tt, :], func=AF.Copy, scale=tT.ap[:, 2, t0 + tt:t0 + tt + 1])
                return i
            P.dve(f_q, r=[tT.n, iota_bf.n], w=[Qb.n])
            P.dve(f_p, r=[tT.n, iota_bf.n], w=[Pb.n])
            P.act(f_pg, r=[tT.n], w=[Pb.n])
            for sub in range(4):
                gb = 1 + gbi[0] % 3; gbi[0] += 1

                def f_g(e, sub=sub, gb=gb):
                    for u in range(4):
                        tt = sub * 4 + u
                        i = e.matmul(bank(gb).rearrange("p (i t) -> p i t", t=4)[:, :, u], lhsT=Qb.ap[:, tt, :], rhs=Pb.ap[:, tt, :],
                                     start=True, stop=True, skip_group_check=True)
                    return i
                P.pe(f_g, r=[Pb.n, Qb.n], w=[psn(gb)])
                ts = t0 + sub * 4
                P.act(lambda e, gb=gb, ts=ts: e.activation(out=Gc.ap[:, :, ts:ts + 4],
                                                           in_=bank(gb).rearrange("p (i t) -> p i t", t=4), func=AF.Copy),
                      r=[], w=[psn(gb), Gc.n])

        def stage_C_store(c):
            P.dma(lambda e: e.dma_start(out=G_d[c].rearrange("j i t -> j (i t)"), in_=Gc.ap.rearrange("j i t -> j (i t)")),
                  r=[Gc.n], w=["G_d.%d" % part_of(c)], q="act")

        G_v = G_d.rearrange("c j i t -> j c i t")
        U_v2 = peer_u.rearrange("(g ii j) d -> g j ii d", ii=2, j=128)
        w_state = {"ga": 0, "mm": 0}

        WB = {"xh": xh, "Gg": Gg, "Un": Un, "UT": UT, "Wt": Wt, "ga": ga}

        def w_group_loads(p, ig):
            Gg_ = WB["Gg"]; s = ig % len(Gg_)
            P.dma(lambda e: e.dma_start(out=Gg_[s].ap[:, 0:nch(p)], in_=G_v[:, PB[p]:PB[p + 1], 4 * ig:4 * ig + 4, :]), r=["G_d.%d" % p], w=[Gg_[s].n])

        def w_U_load(q):
            Un_ = WB["Un"]; s = q % len(Un_)
            P.dma(lambda e: e.dma_start(out=Un_[s].ap, in_=U_v2[q]), w=[Un_[s].n], q="pool")

        def w_part_begin(p):
            xh_ = WB["xh"]
            P.dma(lambda e: e.dma_start(out=xh_.ap[:, :, 0:ntok(p)], in_=xT_d[:, :, tok0(p):tok0(p) + ntok(p)]), r=["xT_d"], w=[xh_.n])
            w_group_loads(p, 0)
            if p == 0:
                w_U_load(0); w_U_load(1)
            if p > 0:
                for i in range(len(WB["UT"]) - 1):
                    w_UT_load(i)

        def w_UT_load(i):
            UT_ = WB["UT"]; u = i % len(UT_)
            P.dma(lambda e: e.dma_start(out=UT_[u].ap, in_=UT_d[i]), r=["UT_d"], w=[UT_[u].n])

        def w_T(i):
            ii = i % 2
            Un_ = WB["Un"]; UT_ = WB["UT"]
            s = (i // 2) % len(Un_)
            u = i % len(UT_)
            P.pe(lambda e: [e.transpose(out=bankb(4, 2)[:, k * 128:(k + 1) * 128], in_=Un_[s].ap[:, ii, k * 128:(k + 1) * 128],
                                        identity=ident_bf.ap) for k in range(16)],
                 r=[Un_[s].n, ident_bf.n], w=[psn(4), psn(5)])
            P.act(lambda e: e.activation(out=UT_[u].ap, in_=bankb(4, 2).rearrange("p (k j) -> p k j", j=128), func=AF.Copy),
                  r=[], w=[psn(4), psn(5), UT_[u].n])
            if NP > 1:
                P.dma(lambda e: e.dma_start(out=UT_d[i], in_=UT_[u].ap), r=[UT_[u].n], w=["UT_d"], q="act")

        def w_block(p, i):
            ig, ii = i // 4, i % 4
            xh_ = WB["xh"]; Gg_ = WB["Gg"]; UT_ = WB["UT"]; Wt_ = WB["Wt"]; ga_ = WB["ga"]
            s = ig % len(Gg_)
            u = i % len(UT_)
            wu = i % len(Wt_)
            if ii == 0 and ig + 1 < 32:
                w_group_loads(p, ig + 1)
            if p == 0:
                if i % 2 == 0 and i // 2 + 2 < 64:
                    w_U_load(i // 2 + 2)
                if i == 0:
                    w_T(0)
                if i + 1 < 128:
                    w_T(i + 1)
            else:
                nxt = i + len(UT_) - 1
                if nxt < 128:
                    w_UT_load(nxt)
            for tg in range(ntok(p) // 512):
                bks = WB.get("banks", [6, 7])
                bk = bks[w_state["mm"] % len(bks)]; w_state["mm"] += 1
                P.pe(lambda e, tg=tg, bk=bk: [e.matmul(bank(bk), lhsT=UT_[u].ap[:, k, :], rhs=xh_.ap[:, k, tg * 512:(tg + 1) * 512],
                                                       start=(k == 0), stop=(k == 15)) for k in range(16)],
                     r=[UT_[u].n, xh_.n], w=[psn(bk)])
                gb_ = ga_[w_state["ga"] % len(ga_)]; w_state["ga"] += 1
                P.act(lambda e, bk=bk, gb_=gb_: e.activation(out=gb_.ap, in_=bank(bk), func=AF.Gelu), r=[], w=[psn(bk), gb_.n])
                P.pool(lambda e, tg=tg, gb_=gb_: e.tensor_tensor(
                    out=Wt_[wu].ap[:, tg * 512:(tg + 1) * 512].rearrange("p (c t) -> p c t", t=128),
                    in0=gb_.ap.rearrange("p (c t) -> p c t", t=128), in1=Gg_[s].ap[:, 4 * tg:4 * tg + 4, ii, :], op=ALU.mult),
                    r=[gb_.n, Gg_[s].n], w=[Wt_[wu].n])
            P.dma(lambda e: e.dma_start(out=W_d[i][:, tok0(p):tok0(p) + ntok(p)], in_=Wt_[wu].ap[:, 0:ntok(p)]), r=[Wt_[wu].n], w=["W_d"], q="pool")

        wnext = {"p": -1, "i": 0}

        def w_emit(n):
            for _ in range(n):
                if wnext["p"] >= 0 and wnext["i"] < 128:
                    w_block(wnext["p"], wnext["i"]); wnext["i"] += 1

        stage_A(0); stage_B(0); stage_Bpe(0)
        for c in range(NT):
            part = part_of(c)
            n_round = 0
            if part >= 1:
                if c == PB[part]:
                    w_part_begin(part - 1)
                    wnext["p"] = part - 1; wnext["i"] = 0
                r_ = c - PB[part] + 1
                n_round = -(-128 * r_ // nch(part)) - wnext["i"]
            n_slots = n_round
            done_ = 0
            for k, t0 in enumerate(range(0, 128, 16)):
                stage_C_group(c, t0)
                tgt = n_slots * (k + 1) // 8
                w_emit(tgt - done_); done_ = tgt
                if c + 1 < NT:
                    stage_A(c + 1, pieces=(k,))
            stage_C_store(c)
            rest = n_round - n_slots
            if c + 1 < NT:
                stage_B(c + 1)
                w_emit(rest * 3 // 4)
                stage_Bpe(c + 1)
                w_emit(rest - rest * 3 // 4)
            else:
                w_emit(rest)
        assert NP == 1 or wnext["i"] == 128, wnext
        if NP > 1:
            P.barrier()
            fb = Bump(AR, A0, END, "X5f")
            TPf = ntok(NP - 1)
            WB = {"xh": fb.alloc("xh", [128, 16, TPf], BF16),
                  "Gg": [fb.alloc("Gg%d" % i, [128, nch(NP - 1), 4, 128], BF16) for i in range(2)],
                  "Un": None,
                  "UT": [fb.alloc("UT%d" % i, [128, 16, 128], BF16) for i in range(6)],
                  "Wt": [fb.alloc("Wt%d" % i, [128, TPf], BF16) for i in range(3)],
                  "ga": [fb.alloc("ga%d" % i, [128, 512], BF16) for i in range(4)],
                  "banks": [2, 3, 4, 5, 6, 7]}
        w_part_begin(NP - 1)
        for i in range(128):
            w_block(NP - 1, i)
        P.barrier()
        if stop <= 7:
            return finish(nc, P, ["W_d", "G_d"])

        hf = AR.view("A.hf", A0, [128, 8, DM], F32)
        bb = Bump(AR, B0, C0, "B5d"); cc = Bump(AR, C0, END, "C5d")
        NB5 = 4
        Wg = [bb.alloc("Wg%d" % i, [128, 4, 1024], BF16) for i in range(NB5)]
        Vg = [bb.alloc("Vg%d" % i, [128, 4, 512], BF16) for i in range(NB5)]
        h2t = [cc.alloc("h2t%d" % i, [128, 512], F32) for i in range(8)]
        gfr = cc.alloc("gfr", [128, DM], F32)
        ot = [cc.alloc("ot%d" % i, [128, DM], F32) for i in range(2)]
        sq5 = cc.alloc("sq5", [128, DM], BF16)
        P.dma(lambda e: e.dma_start(out=gfr.ap, in_=lnf_g.broadcast_to([128, DM])), w=[gfr.n])
        W_v = W_d.rearrange("(g ii) j t -> g j ii t", ii=4)
        V_v = peer_v.rearrange("(g ii j) d -> g j ii d", ii=4, j=128)
        li = [0]
        for th in range(2):
            for dg in range(4):
                for ig in range(32):
                    if ig == 12:
                        for c8 in range(8):
                            cg = th * 8 + c8
                            P.dma(lambda e, cg=cg, dg=dg, c8=c8: e.dma_start(out=h2t[c8].ap, in_=h2_d[cg * 128:(cg + 1) * 128, dg * 512:(dg + 1) * 512]), r=["h2_d"], w=[h2t[c8].n])
                    s = li[0] % NB5; li[0] += 1
                    P.dma(lambda e, ig=ig, s=s, th=th: e.dma_start(out=Wg[s].ap, in_=W_v[ig][:, :, th * 1024:(th + 1) * 1024]), r=["W_d"], w=[Wg[s].n])
                    P.dma(lambda e, ig=ig, s=s, dg=dg: e.dma_start(out=Vg[s].ap, in_=V_v[ig][:, :, dg * 512:(dg + 1) * 512]), w=[Vg[s].n], q="pool")

                    def f_y(e, ig=ig, s=s):
                        for ii in range(4):
                            i = 4 * ig + ii
                            for c8 in range(8):
                                r_ = e.matmul(bank(c8), lhsT=Wg[s].ap[:, ii, c8 * 128:(c8 + 1) * 128], rhs=Vg[s].ap[:, ii, :],
                                              start=(i == 0), stop=(i == 127), skip_group_check=True)
                        return r_
                    P.pe(f_y, r=[Wg[s].n, Vg[s].n], w=[psn(b) for b in range(8)])
                for c8 in range(8):
                    P.dve(lambda e, c8=c8, dg=dg: e.tensor_tensor(out=hf.ap[:, c8, dg * 512:(dg + 1) * 512], in0=bank(c8), in1=h2t[c8].ap, op=ALU.add),
                          r=[h2t[c8].n], w=[psn(c8), hf.n + ".%d" % c8])
            for c8 in range(8):
                cg = th * 8 + c8
                s = c8 % 2
                nm = ["5.ss%d" % cg, "5.t%d" % cg, "5.rs%d" % cg]
                P.act(lambda e, c8=c8, cg=cg: e.activation(out=sq5.ap, in_=hf.ap[:, c8, :], func=AF.Square, accum_out=ssA.ap[:, cg:cg + 1]),
                      r=[hf.n + ".%d" % c8], w=[sq5.n, nm[0]])
                rstd_ops(ssA.ap[:, cg:cg + 1], tA.ap[:, cg:cg + 1], rsA.ap[:, cg:cg + 1], nm, DM)
                P.dve(lambda e, c8=c8, cg=cg, s=s: e.scalar_tensor_tensor(out=ot[s].ap, in0=hf.ap[:, c8, :], scalar=rsA.ap[:, cg:cg + 1], in1=gfr.ap,
                                                                          op0=ALU.mult, op1=ALU.mult),
                      r=[hf.n + ".%d" % c8, nm[2], gfr.n], w=[ot[s].n])
                P.dma(lambda e, cg=cg, s=s: e.dma_start(out=out[cg * 128:(cg + 1) * 128, :], in_=ot[s].ap), r=[ot[s].n], w=["out"], q="act")
        return finish(nc, P, ["out"])


def finish(nc, P, outs):
    P.add("sp", lambda e: None, reads=list(outs) + ["out"])
    P.barrier()
    P.add("sp", lambda e: None)
    P.emit()
    return nc


_CONSTS = None


def make_in_maps(inputs):
    global _CONSTS
    if _CONSTS is None:
        _CONSTS = host_constants()
    f = lambda a: np.ascontiguousarray(np.asarray(a, dtype=np.float32))
    shared = {
        "ln1_g": f(inputs["ln1_g"]).reshape(1, DM), "w_in": f(inputs["w_in"]).reshape(DM, 4160),
        "q_a_norm": f(inputs["q_a_norm"]).reshape(1, 512), "kv_a_norm": f(inputs["kv_a_norm"]).reshape(1, 512),
        "w_uq": f(inputs["w_uq"]).reshape(512, 1536), "w_ukv": f(inputs["w_ukv"]).reshape(512, 2048),
        "out_norm_dil": f(inputs["out_norm_dil"]).reshape(1, 1024), "out_norm_mla": f(inputs["out_norm_mla"]).reshape(1, 1024),
        "w_o": f(inputs["w_o"]).reshape(DM, DM), "ln2_g": f(inputs["ln2_g"]).reshape(1, DM),
        "peer_wq": f(inputs["peer_wq"]).reshape(DM, DM), "peer_sub_keys": f(inputs["peer_sub_keys"]).reshape(16, 128, 128),
        "peer_u": f(inputs["peer_u"]).reshape(16384, DM), "peer_v": f(inputs["peer_v"]).reshape(16384, DM),
        "lnf_g": f(inputs["lnf_g"]).reshape(1, DM),
    }
    shared.update(_CONSTS)
    xx = f(inputs["x"])
    maps = []
    for b in range(8):
        m = dict(shared)
        m["x"] = xx[b]
        maps.append(m)
    return maps


def kernel(**inputs):
    nc = build_program()
    in_maps = make_in_maps(inputs)
    res = run_bass_kernel_spmd(nc, in_maps, core_ids=list(range(8)))
    return np.stack([np.asarray(r["out"], dtype=np.float32) for r in res.results], 0)
```

```python
import contextlib
import numpy as np
import concourse.bass as bass
import concourse.mybir as mybir
from concourse.bass_utils import run_bass_kernel_spmd

F32 = mybir.dt.float32
BF16 = mybir.dt.bfloat16
U32 = mybir.dt.uint32
U8 = mybir.dt.uint8
ALU = mybir.AluOpType
AF = mybir.ActivationFunctionType
AX = mybir.AxisListType

S = 2048
DM = 2048
NT = 16
EPS = 1e-6
NEGBIG = -30000.0
KB = 1024

ENGS = ("pe", "act", "dve", "pool", "sp")
NDMASEM = 8


class Op:
    def __init__(self, eng, fn, reads, writes, dma):
        self.eng = eng; self.fn = fn; self.reads = tuple(reads); self.writes = tuple(writes); self.dma = dma
        self.waits = {}; self.has_dep = False; self.seq = None; self.deps = []


class Prog:
    def __init__(self, nc):
        self.nc = nc
        self.ops = []

    def add(self, eng, fn, reads=(), writes=(), dma=0):
        op = Op(eng, fn, reads, writes, dma)
        self.ops.append(op)
        return op

    def pe(self, fn, r=(), w=()): return self.add("pe", fn, r, w)
    def act(self, fn, r=(), w=()): return self.add("act", fn, r, w)
    def dve(self, fn, r=(), w=()): return self.add("dve", fn, r, w)
    def pool(self, fn, r=(), w=()): return self.add("pool", fn, r, w)
    def dma(self, fn, r=(), w=(), n=1, q="sp"): return self.add(q, fn, r, w, dma=n)
    def barrier(self): return self.add("bar", None)

    def analyze(self):
        last_w = {}; readers = {}
        dcount = {}; dtotal = {}
        last_op = {e: None for e in ENGS}
        for op in self.ops:
            if op.eng == "bar":
                for e in ENGS:
                    lo = last_op[e]
                    if lo is not None and not lo.dma and lo.eng != "sp":
                        lo.has_dep = True
                last_w = {}; readers = {}
                continue
            if op.dma:
                q = op.eng
                k = dcount.get(q, 0) % NDMASEM
                dcount[q] = dcount.get(q, 0) + 1
                op.sem = ("dma", q, k)
                op.prev_target = dtotal.get((q, k), 0)
                op.target = op.prev_target + 16 * op.dma
                dtotal[(q, k)] = op.target
            else:
                op.sem = ("eng", op.eng)
                last_op[op.eng] = op
            deps = []
            for r in op.reads:
                if r in last_w: deps.append(last_w[r])
            for w in op.writes:
                if w in last_w: deps.append(last_w[w])
                deps.extend(readers.get(w, ()))
            op.deps = []
            for d in deps:
                if d is op: continue
                if (not d.dma) and (not op.dma) and d.eng == "pe" and op.eng == "pe":
                    continue
                d.has_dep = True
                op.deps.append(d)
            for r in op.reads:
                readers.setdefault(r, []).append(op)
            for w in op.writes:
                last_w[w] = op
                readers[w] = []
        seq = {e: 0 for e in ENGS}
        for op in self.ops:
            if op.eng != "bar" and not op.dma and op.has_dep:
                seq[op.eng] += 1
                op.seq = seq[op.eng]
        waited = {e: {} for e in ENGS}
        pending = {e: {} for e in ENGS}
        cur_seq = {e: 0 for e in ENGS}
        cur_dma = {}
        for op in self.ops:
            if op.eng == "bar":
                need = {("eng", e): cur_seq[e] for e in ENGS if cur_seq[e] > 0}
                need.update(cur_dma)
                for e in ENGS:
                    for s, v in need.items():
                        pending[e][s] = max(pending[e].get(s, 0), v)
                continue
            need = dict(pending[op.eng]); pending[op.eng] = {}
            if op.dma and op.prev_target > 0:
                need[op.sem] = max(need.get(op.sem, 0), op.prev_target)
            for d in op.deps:
                v = d.target if d.dma else d.seq
                need[d.sem] = max(need.get(d.sem, 0), v)
            w = waited[op.eng]
            op.waits = {}
            for s, v in need.items():
                if w.get(s, 0) < v:
                    op.waits[s] = v
                    w[s] = v
            if op.dma:
                cur_dma[op.sem] = op.target
            elif op.seq is not None:
                cur_seq[op.eng] = op.seq

    def emit(self):
        nc = self.nc
        self.analyze()
        with contextlib.ExitStack() as st:
            sems = {}
            for e in ("pe", "act", "dve", "pool"):
                sems[("eng", e)] = st.enter_context(nc.semaphore("s_" + e))
            for q in ("sp", "pool", "act"):
                for k in range(NDMASEM):
                    sems[("dma", q, k)] = st.enter_context(nc.semaphore(f"d_{q}{k}"))
            block = st.enter_context(nc.Block())
            engmap = {"pe": "tensor", "act": "scalar", "dve": "vector", "pool": "gpsimd", "sp": "sync"}
            ops = self.ops

            def mk(ename):
                def body(eng):
                    for op in ops:
                        if op.eng != ename: continue
                        for s, v in op.waits.items():
                            eng.wait_ge(sems[s], v)
                        ins = op.fn(eng)
                        if ins is None: continue
                        if op.dma:
                            if not isinstance(ins, (list, tuple)): ins = [ins]
                            assert len(ins) == op.dma, (len(ins), op.dma)
                            for i in ins: i.then_inc(sems[op.sem], 16)
                        elif op.has_dep:
                            if isinstance(ins, (list, tuple)): ins = ins[-1]
                            ins.then_inc(sems[op.sem], 1)
                return body
            for ename, attr in engmap.items():
                getattr(block, attr)(mk(ename))


class Buf:
    def __init__(self, ap, name): self.ap = ap; self.n = name


ESZ = {F32: 4, BF16: 2, U32: 4}


class Arena:
    def __init__(self, tens):
        self.t = tens
        self.cnt = 0

    def view(self, name, off, shape, dtype):
        n = int(np.prod(shape[1:]))
        ap = self.t[0:shape[0], off:off + n * ESZ[dtype]].bitcast(dtype)
        if len(shape) > 2:
            names = ["a%d" % i for i in range(len(shape) - 1)]
            pat = "p (" + " ".join(names) + ") -> p " + " ".join(names)
            ap = ap.rearrange(pat, **{nm: int(sz) for nm, sz in zip(names[1:], shape[2:])})
        self.cnt += 1
        return Buf(ap, name)

    @staticmethod
    def nbytes(shape, dtype):
        return int(np.prod(shape[1:])) * ESZ[dtype]


class Bump:
    def __init__(self, arena, base, limit, tag):
        self.a = arena; self.base = base; self.limit = limit; self.off = base; self.tag = tag

    def alloc(self, name, shape, dtype):
        nb = (Arena.nbytes(shape, dtype) + 31) // 32 * 32
        assert self.off + nb <= self.limit, (self.tag, name, self.off, nb, self.limit)
        b = self.a.view(self.tag + "." + name, self.off, shape, dtype)
        self.off += nb
        return b


def host_constants():
    k = np.arange(128)[:, None, None]
    db = np.arange(16)[None, :, None]
    q = np.arange(128)[None, None, :]
    dist = 128 * db + q - k
    mult = ((dist >= 0) & (dist <= 128)).astype(np.float64) + ((dist >= 0) & (dist % 4 == 0) & (dist <= 512)) \
        + ((dist >= 0) & (dist % 16 == 0) & (dist <= 2048))
    lnm = np.where(mult > 0, np.log(np.maximum(mult, 1.0)), NEGBIG).astype(np.float32)
    negd = np.where(mult > 0, -dist, 0).astype(np.float32)
    c_dil = np.stack([negd.reshape(128, 2048), lnm.reshape(128, 2048)], 0).astype(np.float32)
    kk = np.arange(128)[:, None]; qq = np.arange(128)[None, :]
    c_tri = np.where(kk <= qq, 0.0, NEGBIG).astype(np.float32)
    half = 32
    freqs = (np.float32(10000.0) ** (-np.arange(half, dtype=np.float32) / np.float32(half))).astype(np.float32)
    ang = (np.arange(S, dtype=np.float32)[:, None] * freqs[None, :]).astype(np.float32)
    cos = np.cos(ang).astype(np.float32).T; sin = np.sin(ang).astype(np.float32).T
    c_rope = np.stack([np.concatenate([cos, cos], 0), np.concatenate([sin, sin], 0)], 0)
    return {"c_dil": c_dil, "c_tri": c_tri, "c_rope": np.ascontiguousarray(c_rope.astype(np.float32))}


PART_BOUNDS = [0, 8, 16]
SLOPES = [2.0 ** (-8.0 * (i + 1) / 8) for i in range(8)]


def build_program(dbg=(), stop=99):
    nc = bass.Bass("TRN2", target_bir_lowering=False)

    def din(name, shape, dtype=F32):
        return nc.dram_tensor(name, list(shape), dtype, kind="ExternalInput").ap()

    def dscr(name, shape, dtype):
        kind = "ExternalOutput" if name in dbg else "Internal"
        return nc.dram_tensor(name, list(shape), dtype, kind=kind).ap()

    x = din("x", [S, DM])
    ln1_g = din("ln1_g", [1, DM]); w_in = din("w_in", [DM, 4160])
    q_a_norm = din("q_a_norm", [1, 512]); kv_a_norm = din("kv_a_norm", [1, 512])
    w_uq = din("w_uq", [512, 1536]); w_ukv = din("w_ukv", [512, 2048])
    out_norm_dil = din("out_norm_dil", [1, 1024]); out_norm_mla = din("out_norm_mla", [1, 1024])
    w_o = din("w_o", [DM, DM]); ln2_g = din("ln2_g", [1, DM]); peer_wq = din("peer_wq", [DM, DM])
    sub_keys = din("peer_sub_keys", [16, 128, 128])
    peer_u = din("peer_u", [16384, DM]); peer_v = din("peer_v", [16384, DM]); lnf_g = din("lnf_g", [1, DM])
    c_dil = din("c_dil", [2, 128, 2048]); c_tri = din("c_tri", [128, 128]); c_rope = din("c_rope", [2, 64, S])
    out = nc.dram_tensor("out", [S, DM], F32, kind="ExternalOutput").ap()

    qaT_d = dscr("qaT_d", [8, 128, S], BF16); kaT_d = dscr("kaT_d", [8, 128, S], BF16); va_d = dscr("va_d", [8, S, 128], BF16)
    qnT_d = dscr("qnT_d", [8, 128, S], BF16); qpT_d = dscr("qpT_d", [8, 64, S], BF16)
    knT_d = dscr("knT_d", [8, 128, S], BF16); kpT_d = dscr("kpT_d", [64, S], BF16); vm_d = dscr("vm_d", [8, S, 128], BF16)
    h2_d = dscr("h2_d", [S, DM], F32)
    xT_d = dscr("xT_d", [128, 16, S], BF16)
    G_d = dscr("G_d", [16, 128, 128, 128], BF16)
    W_d = dscr("W_d", [128, 128, S], BF16)
    onT_d = dscr("onT_d", [128, 16, S], BF16)
    dbg_d = dscr("dbg_d", [128, 4096], F32)
    sc_d = dscr("sc_d", [16, 128, S], F32)
    UT_d = dscr("UT_d", [128, 128, 16, 128], BF16)

    with contextlib.ExitStack() as st:
        arena_t = st.enter_context(nc.sbuf_tensor("arena", [128, 204 * KB], U8))
        ps_t = st.enter_context(nc.psum_tensor("ps", [128, 4096], F32))
        st.enter_context(nc.allow_non_contiguous_dma("small strided parameter loads"))
        AR = Arena(arena_t)
        P = Prog(nc)

        def bank(i, n=1):
            return ps_t[:, i * 512:(i + n) * 512]

        def bankb(i, n=1):
            return ps_t[:, i * 512:(i + n) * 512].bitcast(BF16)

        def psn(i): return "ps%d" % i

        CONST0, A0, B0, C0, END = 0, 8 * KB, 72 * KB, 136 * KB, 204 * KB
        cb = Bump(AR, CONST0, A0, "K")
        ident_bf = cb.alloc("ident_bf", [128, 128], BF16)
        ident_f = cb.alloc("ident_f", [128, 128], F32)
        ones_bf = cb.alloc("ones_bf", [128, 128], BF16)
        iota_bf = cb.alloc("iota_bf", [128, 128], BF16)
        iota16 = cb.alloc("iota16", [128, 16], F32)
        iota_f = cb.alloc("iota_f", [128, 128], F32)
        mhalf = cb.alloc("mhalf", [128, 1], F32)
        ssA = cb.alloc("ssA", [128, 16], F32); tA = cb.alloc("tA", [128, 16], F32); rsA = cb.alloc("rsA", [128, 16], F32)
        gcol = cb.alloc("gcol", [128, 16], F32)
        keysT = cb.alloc("keysT", [128, 16, 128], BF16)

        P.pool(lambda e: e.memset(ident_bf.ap, 0.0), w=[ident_bf.n])
        P.pool(lambda e: e.affine_select(out=ident_bf.ap, in_=ident_bf.ap, pattern=[[-1, 128]], compare_op=ALU.not_equal,
                                         fill=1.0, base=0, channel_multiplier=1), r=[ident_bf.n], w=[ident_bf.n])
        P.pool(lambda e: e.memset(ident_f.ap, 0.0), w=[ident_f.n])
        P.pool(lambda e: e.affine_select(out=ident_f.ap, in_=ident_f.ap, pattern=[[-1, 128]], compare_op=ALU.not_equal,
                                         fill=1.0, base=0, channel_multiplier=1), r=[ident_f.n], w=[ident_f.n])
        P.pool(lambda e: e.memset(ones_bf.ap, 1.0), w=[ones_bf.n])
        P.pool(lambda e: e.iota(iota_f.ap, pattern=[[1, 128]], base=0, channel_multiplier=0, allow_small_or_imprecise_dtypes=True), w=[iota_f.n])
        P.pool(lambda e: e.iota(iota16.ap, pattern=[[1, 16]], base=0, channel_multiplier=0, allow_small_or_imprecise_dtypes=True), w=[iota16.n])
        P.dve(lambda e: e.tensor_copy(out=iota_bf.ap, in_=iota_f.ap), r=[iota_f.n], w=[iota_bf.n])
        P.dve(lambda e: e.memset(mhalf.ap, -0.5), w=[mhalf.n])

        def rstd_ops(ss_ap, t_ap, rs_ap, names, n_feat):
            P.dve(lambda e: e.tensor_scalar(out=t_ap, in0=ss_ap, scalar1=1.0 / n_feat, scalar2=EPS, op0=ALU.mult, op1=ALU.add),
                  r=[names[0]], w=[names[1]])
            P.pool(lambda e: e.tensor_tensor(out=rs_ap, in0=t_ap, in1=mhalf.ap.to_broadcast(list(t_ap.shape)), op=ALU.pow),
                   r=[names[1], mhalf.n], w=[names[2]])

        def norm_T(tag, src, g_dram, xT):
            cbp = Bump(AR, C0, END, tag)
            xs = [cbp.alloc("xs%d" % i, [128, DM], F32) for i in range(4)]
            xnb = [cbp.alloc("xnb%d" % i, [128, DM], BF16) for i in range(2)]
            sq = cbp.alloc("sq", [128, DM], BF16)
            grep = cbp.alloc("grep", [128, DM], F32)
            P.dma(lambda e: e.dma_start(out=grep.ap, in_=g_dram.broadcast_to([128, DM])), w=[grep.n])
            def st1(c):
                s4 = c % 4
                P.dma(lambda e: e.dma_start(out=xs[s4].ap, in_=src[c * 128:(c + 1) * 128, :]), w=[xs[s4].n])
                nm = [tag + ".ss%d" % c, tag + ".t%d" % c, tag + ".rs%d" % c]
                P.act(lambda e: e.activation(out=sq.ap, in_=xs[s4].ap, func=AF.Square, accum_out=ssA.ap[:, c:c + 1]),
                      r=[xs[s4].n], w=[sq.n, nm[0]])
                rstd_ops(ssA.ap[:, c:c + 1], tA.ap[:, c:c + 1], rsA.ap[:, c:c + 1], nm, DM)

            def st2(c):
                s = c % 2; s4 = c % 4; b0 = 2 * s
                P.dve(lambda e: e.scalar_tensor_tensor(out=xnb[s].ap, in0=xs[s4].ap, scalar=rsA.ap[:, c:c + 1], in1=grep.ap,
                                                       op0=ALU.mult, op1=ALU.mult),
                      r=[xs[s4].n, tag + ".rs%d" % c, grep.n], w=[xnb[s].n])
                P.pe(lambda e: [e.transpose(out=bankb(b0, 2)[:, k * 128:(k + 1) * 128], in_=xnb[s].ap[:, k * 128:(k + 1) * 128],
                                            identity=ident_bf.ap) for k in range(16)],
                     r=[xnb[s].n, ident_bf.n], w=[psn(b0), psn(b0 + 1)])

            def st3(c):
                b0 = 2 * (c % 2)
                P.act(lambda e: e.activation(out=xT.ap[:, :, c * 128:(c + 1) * 128],
                                             in_=bankb(b0, 2).rearrange("p (k t) -> p k t", t=128), func=AF.Copy),
                      r=[], w=[psn(b0), psn(b0 + 1), xT.n + ".c%d" % c])

            for c in range(NT + 2):
                if c < NT: st1(c)
                if 0 <= c - 1 < NT: st2(c - 1)
                if 0 <= c - 2 < NT: st3(c - 2)

        def xT_reads(xT): return [xT.n + ".c%d" % c for c in range(NT)]

        def load_w(dst, wsrc, c0, n, K):
            src = wsrc.rearrange("(k p) c -> p k c", p=128)[:, :, c0:c0 + n]
            P.dma(lambda e: e.dma_start(out=dst.ap[:, 0:K, 0:n], in_=src), w=[dst.n], q="pool")

        evac_rr = [0]

        def evac(out_ap, in_ap, r, w):
            evac_rr[0] ^= 1
            if evac_rr[0]:
                P.act(lambda e: e.activation(out=out_ap, in_=in_ap, func=AF.Copy), r=r, w=w)
            else:
                P.dve(lambda e: e.tensor_copy(out=out_ap, in_=in_ap), r=r, w=w)

        xnT = AR.view("A.xT", A0, [128, 16, S], BF16)
        norm_T("n1", x, ln1_g, xnT)
        P.barrier()
        if stop <= 1:
            P.dma(lambda e: e.dma_start(out=xT_d, in_=xnT.ap), r=[], w=["xT_d"])
            return finish(nc, P, ["xT_d"])

        bb = Bump(AR, B0, C0, "B2"); cc = Bump(AR, C0, END, "C2")
        cqnT = bb.alloc("cqnT", [128, 4, S], BF16); ckvnT = bb.alloc("ckvnT", [128, 4, S], BF16)
        rope_sb = bb.alloc("rope", [64, 2, S], F32)
        wkr = bb.alloc("wkr", [128, 16, 64], BF16); wkrs = bb.alloc("wkrs", [128, 16, 64], BF16)
        gq = [bb.alloc("gq%d" % i, [128, 512], F32) for i in range(2)]
        wb = [cc.alloc("wb%d" % i, [128, 16, 512], BF16) for i in range(2)]
        stg = [cc.alloc("stg%d" % i, [128, S], BF16) for i in range(2)]
        sq2 = cc.alloc("sq", [128, 512], BF16)
        cqn = [cc.alloc("cqn%d" % i, [128, 512], BF16) for i in range(2)]
        rt = [cc.alloc("rt%d" % i, [64, 512], F32) for i in range(3)]
        P.dma(lambda e: e.dma_start(out=rope_sb.ap, in_=c_rope.rearrange("a p t -> p a t")), w=[rope_sb.n])
        P.dma(lambda e: e.dma_start(out=gq[0].ap, in_=q_a_norm.broadcast_to([128, 512])), w=[gq[0].n])
        P.dma(lambda e: e.dma_start(out=gq[1].ap, in_=kv_a_norm.broadcast_to([128, 512])), w=[gq[1].n])
        xr = xT_reads(xnT)
        psrr = [0]

        def next_bank():
            psrr[0] = (psrr[0] + 1) % 4
            return 4 + psrr[0]

        def mm_fm(wt, col0, M, rhsT, K, tg, bk, rnames):
            P.pe(lambda e: [e.matmul(bank(bk)[0:M, :], lhsT=wt.ap[:, k, col0:col0 + M], rhs=rhsT.ap[:, k, tg * 512:(tg + 1) * 512],
                                     start=(k == 0), stop=(k == K - 1)) for k in range(K)],
                 r=rnames, w=[psn(bk)])

        def mm_tm(lhsT_buf, c, wt, rhs_fn, K, bk, rnames):
            P.pe(lambda e: [e.matmul(bank(bk), lhsT=lhsT_buf.ap[:, k, c * 128:(c + 1) * 128], rhs=rhs_fn(k),
                                     start=(k == 0), stop=(k == K - 1)) for k in range(K)],
                 r=rnames, w=[psn(bk)])

        cqbank = {}
        for which, (col0, dstT, g) in enumerate([(3072, cqnT, gq[0]), (3584, ckvnT, gq[1])]):
            w = wb[which]
            load_w(w, w_in, col0, 512, 16)

            def q1a(c, w=w, which=which):
                bk = 4 + (c % 4)
                cqbank[(which, c)] = bk
                mm_tm(xnT, c, w, (lambda k, w=w: w.ap[:, k, :]), 16, bk, [xnT.n + ".c%d" % c, w.n])

            def q1b(c, which=which):
                bk = cqbank[(which, c)]
                nm = ["2.ss%d" % c, "2.t%d" % c, "2.rs%d" % c]
                P.act(lambda e: e.activation(out=sq2.ap, in_=bank(bk), func=AF.Square, accum_out=ssA.ap[:, c:c + 1]),
                      r=[], w=[psn(bk), sq2.n, nm[0]])
                rstd_ops(ssA.ap[:, c:c + 1], tA.ap[:, c:c + 1], rsA.ap[:, c:c + 1], nm, 512)

            def q2(c, g=g, which=which):
                bk = cqbank[(which, c)]
                s = c % 2
                P.dve(lambda e: e.scalar_tensor_tensor(out=cqn[s].ap, in0=bank(bk), scalar=rsA.ap[:, c:c + 1], in1=g.ap,
                                                       op0=ALU.mult, op1=ALU.mult),
                      r=["2.rs%d" % c, g.n], w=[psn(bk), cqn[s].n])
                P.pe(lambda e: [e.transpose(out=bankb(s)[:, k * 128:(k + 1) * 128], in_=cqn[s].ap[:, k * 128:(k + 1) * 128],
                                            identity=ident_bf.ap) for k in range(4)],
                     r=[cqn[s].n, ident_bf.n], w=[psn(s)])

            def q3(c, dstT=dstT):
                tb = c % 2
                evac(dstT.ap[:, :, c * 128:(c + 1) * 128], bankb(tb)[:, 0:512].rearrange("p (k t) -> p k t", t=128),
                     [], [psn(tb), dstT.n + ".c%d" % c])

            for c in range(NT + 2):
                if c < NT: q1a(c)
                if 0 <= c - 1 < NT: q2(c - 1)
                if c < NT: q1b(c)
                if 0 <= c - 2 < NT: q3(c - 2)
        P.dma(lambda e: e.dma_start(out=wkr.ap, in_=w_in.rearrange("(k p) c -> p k c", p=128)[:, :, 4096:4160]), w=[wkr.n], q="pool")
        P.dve(lambda e: e.tensor_scalar(out=wkrs.ap[:, :, 0:32], in0=wkr.ap[:, :, 32:64], scalar1=-1.0, scalar2=None, op0=ALU.mult),
              r=[wkr.n], w=[wkrs.n + ".a"])
        P.dve(lambda e: e.tensor_copy(out=wkrs.ap[:, :, 32:64], in_=wkr.ap[:, :, 0:32]), r=[wkr.n], w=[wkrs.n + ".b"])

        def rope_fm(wA, wAn, wB, wBn, colA, colB, rhsT, K, rnames, dst_stage):
            rt0, rt1 = rt[0], rt[1]
            for tg in range(4):
                bA = next_bank()
                mm_fm(wA, colA, 64, rhsT, K, tg, bA, rnames + wAn)
                bB = next_bank()
                mm_fm(wB, colB, 64, rhsT, K, tg, bB, rnames + wBn)
                P.dve(lambda e, bA=bA, tg=tg, rt0=rt0: e.tensor_tensor(out=rt0.ap, in0=bank(bA)[0:64, :], in1=rope_sb.ap[:, 0, tg * 512:(tg + 1) * 512], op=ALU.mult),
                      r=[rope_sb.n], w=[psn(bA), rt0.n])
                P.dve(lambda e, bB=bB, tg=tg, rt1=rt1: e.tensor_tensor(out=rt1.ap, in0=bank(bB)[0:64, :], in1=rope_sb.ap[:, 1, tg * 512:(tg + 1) * 512], op=ALU.mult),
                      r=[rope_sb.n], w=[psn(bB), rt1.n])
                P.dve(lambda e, tg=tg, rt0=rt0, rt1=rt1: e.tensor_tensor(out=dst_stage.ap[0:64, tg * 512:(tg + 1) * 512], in0=rt0.ap, in1=rt1.ap, op=ALU.add),
                      r=[rt0.n, rt1.n], w=[dst_stage.n])

        rope_fm(wkr, [wkr.n], wkrs, [wkrs.n + ".a", wkrs.n + ".b"], 0, 0, xnT, 16, xr, stg[0])
        P.dma(lambda e: e.dma_start(out=kpT_d, in_=stg[0].ap[0:64, :]), r=[stg[0].n], w=["kpT_d"], q="act")
        sgi = [0]
        wbi = [0]
        for (col0, dst) in [(0, qaT_d), (1024, kaT_d)]:
            for half in range(2):
                w = wb[wbi[0] % 2]; wbi[0] += 1
                load_w(w, w_in, col0 + half * 512, 512, 16)
                for m in range(4):
                    h = half * 4 + m
                    sg = stg[sgi[0] % 2]; sgi[0] += 1
                    for tg in range(4):
                        bk = next_bank()
                        mm_fm(w, m * 128, 128, xnT, 16, tg, bk, xr + [w.n])
                        evac(sg.ap[:, tg * 512:(tg + 1) * 512], bank(bk), [], [psn(bk), sg.n])
                    P.dma(lambda e, sg=sg, dst=dst, h=h: e.dma_start(out=dst[h], in_=sg.ap), r=[sg.n], w=["qk_d"], q="act")
        for half in range(2):
            w = wb[wbi[0] % 2]; wbi[0] += 1
            load_w(w, w_in, 2048 + half * 512, 512, 16)
            for c in range(NT):
                bk = next_bank()
                mm_tm(xnT, c, w, (lambda k, w=w: w.ap[:, k, :]), 16, bk, [xnT.n + ".c%d" % c, w.n])
                sg = stg[sgi[0] % 2]; sgi[0] += 1
                evac(sg.ap[:, 0:512], bank(bk), [], [psn(bk), sg.n])
                P.dma(lambda e, sg=sg, c=c, half=half: e.dma_start(
                    out=va_d[half * 4:(half + 1) * 4, c * 128:(c + 1) * 128, :].rearrange("h t e -> t h e"),
                    in_=sg.ap[:, 0:512].rearrange("p (h e) -> p h e", e=128)), r=[sg.n], w=["va_d"], q="act")
        P.barrier()
        cc = Bump(AR, C0, END, "C2b")
        wuq = cc.alloc("wuq", [128, 4, 1536], BF16); wuqs = cc.alloc("wuqs", [128, 4, 8, 64], BF16)
        wukv = cc.alloc("wukv", [128, 4, 2048], BF16)
        stg = [cc.alloc("stg%d" % i, [128, S], BF16) for i in range(2)]
        rt = [cc.alloc("rt%d" % i, [64, 512], F32) for i in range(2)]
        load_w(wuq, w_uq, 0, 1536, 4)
        load_w(wukv, w_ukv, 0, 2048, 4)
        wuq_v = wuq.ap.rearrange("p k (h c) -> p k h c", c=192)
        for k in range(4):
            P.dve(lambda e, k=k: e.tensor_scalar(out=wuqs.ap[:, k, :, 0:32], in0=wuq_v[:, k, :, 160:192], scalar1=-1.0, scalar2=None, op0=ALU.mult),
                  r=[wuq.n], w=[wuqs.n + ".a%d" % k])
            P.dve(lambda e, k=k: e.tensor_copy(out=wuqs.ap[:, k, :, 32:64], in_=wuq_v[:, k, :, 128:160]), r=[wuq.n], w=[wuqs.n + ".b%d" % k])
        wuqs_n = [wuqs.n + ".a%d" % k for k in range(4)] + [wuqs.n + ".b%d" % k for k in range(4)]
        wuqs_flat = Buf(wuqs.ap.rearrange("p k h c -> p k (h c)"), wuqs.n)
        cqr = xT_reads(cqnT); ckr = xT_reads(ckvnT)
        sgi = [0]
        for h in range(8):
            sg = stg[sgi[0] % 2]; sgi[0] += 1
            for tg in range(4):
                bk = next_bank()
                mm_fm(wuq, h * 192, 128, cqnT, 4, tg, bk, cqr + [wuq.n])
                evac(sg.ap[:, tg * 512:(tg + 1) * 512], bank(bk), [], [psn(bk), sg.n])
            P.dma(lambda e, sg=sg, h=h: e.dma_start(out=qnT_d[h], in_=sg.ap), r=[sg.n], w=["qnT_d"], q="act")
            sg = stg[sgi[0] % 2]; sgi[0] += 1
            rope_fm(wuq, [wuq.n], wuqs_flat, wuqs_n, h * 192 + 128, h * 64, cqnT, 4, cqr, sg)
            P.dma(lambda e, sg=sg, h=h: e.dma_start(out=qpT_d[h], in_=sg.ap[0:64, :]), r=[sg.n], w=["qpT_d"], q="act")
            sg = stg[sgi[0] % 2]; sgi[0] += 1
            for tg in range(4):
                bk = next_bank()
                mm_fm(wukv, h * 256, 128, ckvnT, 4, tg, bk, ckr + [wukv.n])
                evac(sg.ap[:, tg * 512:(tg + 1) * 512], bank(bk), [], [psn(bk), sg.n])
            P.dma(lambda e, sg=sg, h=h: e.dma_start(out=knT_d[h], in_=sg.ap), r=[sg.n], w=["knT_d"], q="act")
        wukv_v = wukv.ap.rearrange("p k (h c) -> p k h c", c=256)
        for half in range(2):
            for c in range(NT):
                bk = next_bank()
                mm_tm(ckvnT, c, wukv, (lambda k, half=half: wukv_v[:, k, half * 4:(half + 1) * 4, 128:256]), 4, bk,
                      [ckvnT.n + ".c%d" % c, wukv.n])
                sg = stg[sgi[0] % 2]; sgi[0] += 1
                evac(sg.ap[:, 0:512], bank(bk), [], [psn(bk), sg.n])
                P.dma(lambda e, sg=sg, c=c, half=half: e.dma_start(
                    out=vm_d[half * 4:(half + 1) * 4, c * 128:(c + 1) * 128, :].rearrange("h t e -> t h e"),
                    in_=sg.ap[:, 0:512].rearrange("p (h e) -> p h e", e=128)), r=[sg.n], w=["vm_d"], q="act")
        P.barrier()
        if stop <= 2:
            return finish(nc, P, [])

        onT = AR.view("A.onT", A0, [128, 8, S], F32)
        onrm = [AR.view("B.onrm%d" % i, B0 + i * 32 * KB, [128, 8, S], BF16) for i in range(2)]
        tb_ = Bump(AR, B0 + 32 * KB, C0, "B3t")
        negd = tb_.alloc("negd", [128, S], F32); lnm = tb_.alloc("lnm", [128, S], F32)
        cc = Bump(AR, C0, END, "C3")
        qT = [cc.alloc("qT%d" % i, [128, S], BF16) for i in range(2)]
        kT = [cc.alloc("kT%d" % i, [128, S], BF16) for i in range(2)]
        vv = [cc.alloc("v%d" % i, [128, 16, 128], BF16) for i in range(2)]
        qpT = [cc.alloc("qpT%d" % i, [64, S], BF16) for i in range(2)]
        kpT = cc.alloc("kpT", [64, S], BF16)
        biasb = [cc.alloc("bias%d" % i, [128, S], F32) for i in range(2)]
        tri = cc.alloc("tri", [128, 128], F32)
        sp_ = [cc.alloc("sp%d" % i, [128, 512], F32) for i in range(2)]
        pT = [cc.alloc("pT%d" % i, [128, 512], BF16) for i in range(3)]
        rl = cc.alloc("rl", [128, 512], F32)
        sq3 = [cc.alloc("sq%d" % i, [128, 512], BF16) for i in range(2)]
        rsb = cc.alloc("rsb", [128, 512], F32); tt3 = cc.alloc("tt3", [128, 512], F32)
        epsb = cb.alloc("epsb", [128, 1], F32)
        P.dve(lambda e: e.memset(epsb.ap, EPS), w=[epsb.n])
        P.dma(lambda e: e.dma_start(out=negd.ap, in_=c_dil[0]), w=[negd.n])
        P.dma(lambda e: e.dma_start(out=lnm.ap, in_=c_dil[1]), w=[lnm.n])
        P.dma(lambda e: e.dma_start(out=tri.ap, in_=c_tri), w=[tri.n])
        P.dma(lambda e: e.dma_start(out=kpT.ap, in_=kpT_d), w=[kpT.n])

        cnt = {"sp": 0, "pT": 0}
        for grp in range(2):
            scale = (128 ** -0.5) if grp == 0 else (192 ** -0.5)
            gdram = out_norm_dil if grp == 0 else out_norm_mla
            P.dma(lambda e, gdram=gdram, grp=grp: e.dma_start(out=gcol.ap[:, grp * 8:(grp + 1) * 8], in_=gdram.rearrange("o (h p) -> p (o h)", p=128)),
                  w=[gcol.n + "%d" % grp])

            def head_loads(h, grp=grp):
                hb = h % 2
                if grp == 0:
                    P.dma(lambda e: e.dma_start(out=qT[hb].ap, in_=qaT_d[h]), w=[qT[hb].n])
                    P.dma(lambda e: e.dma_start(out=kT[hb].ap, in_=kaT_d[h]), w=[kT[hb].n])
                    P.dma(lambda e: e.dma_start(out=vv[hb].ap, in_=va_d[h].rearrange("(kb p) e -> p kb e", p=128)), w=[vv[hb].n])
                    P.dve(lambda e: e.scalar_tensor_tensor(out=biasb[hb].ap, in0=negd.ap, scalar=float(SLOPES[h]), in1=lnm.ap,
                                                           op0=ALU.mult, op1=ALU.add), r=[negd.n, lnm.n], w=[biasb[hb].n])
                else:
                    P.dma(lambda e: e.dma_start(out=qT[hb].ap, in_=qnT_d[h]), w=[qT[hb].n])
                    P.dma(lambda e: e.dma_start(out=kT[hb].ap, in_=knT_d[h]), w=[kT[hb].n])
                    P.dma(lambda e: e.dma_start(out=vv[hb].ap, in_=vm_d[h].rearrange("(kb p) e -> p kb e", p=128)), w=[vv[hb].n])
                    P.dma(lambda e: e.dma_start(out=qpT[hb].ap, in_=qpT_d[h]), w=[qpT[hb].n])

            steps = []
            for h in range(8):
                for qg in range(4):
                    nkb = 4 * qg + 4
                    for kb in range(nkb):
                        steps.append((h, qg, kb, nkb))

            def geom(st_):
                h, qg, kb, nkb = st_
                q0 = max(qg * 512, kb * 128)
                n = (qg + 1) * 512 - q0
                return h, qg, kb, nkb, q0, n, q0 - qg * 512

            def emit_qk(si, grp=grp):
                h, qg, kb, nkb, q0, n, c0 = geom(steps[si])
                hb = h % 2; sb = SBK[si % 4]
                rn = [qT[hb].n, kT[hb].n] + ([qpT[hb].n, kpT.n] if grp else [])

                def f_qk(e):
                    i = e.matmul(bank(sb)[:, 0:n], lhsT=kT[hb].ap[:, kb * 128:(kb + 1) * 128], rhs=qT[hb].ap[:, q0:q0 + n],
                                 start=True, stop=(grp == 0))
                    if grp:
                        i = e.matmul(bank(sb)[:, 0:n], lhsT=kpT.ap[:, kb * 128:(kb + 1) * 128], rhs=qpT[hb].ap[:, q0:q0 + n],
                                     start=False, stop=True)
                    return i
                P.pe(f_qk, r=rn, w=[psn(sb)])

            def emit_sm(si, grp=grp, scale=scale):
                h, qg, kb, nkb, q0, n, c0 = geom(steps[si])
                hb = h % 2; sb = SBK[si % 4]
                pt = pT[cnt["pT"] % 3]; cnt["pT"] += 1
                diag = kb >= 4 * qg
                if grp == 0:
                    spb = sp_[cnt["sp"] % 2]; cnt["sp"] += 1
                    db0 = q0 // 128 - kb
                    P.dve(lambda e: e.scalar_tensor_tensor(
                        out=spb.ap[:, 0:n], in0=bank(sb)[:, 0:n], scalar=float(scale), in1=biasb[hb].ap[:, db0 * 128:db0 * 128 + n],
                        op0=ALU.mult, op1=ALU.add), r=[biasb[hb].n], w=[psn(sb), spb.n])
                    P.act(lambda e: e.activation(out=pt.ap[:, 0:n], in_=spb.ap[:, 0:n], func=AF.Exp), r=[spb.n], w=[pt.n])
                else:
                    if diag:
                        spb = sp_[cnt["sp"] % 2]; cnt["sp"] += 1
                        P.dve(lambda e: e.scalar_tensor_tensor(
                            out=spb.ap[:, 0:128], in0=bank(sb)[:, 0:128], scalar=float(scale), in1=tri.ap,
                            op0=ALU.mult, op1=ALU.add), r=[tri.n], w=[psn(sb), spb.n])
                        P.act(lambda e: e.activation(out=pt.ap[:, 0:128], in_=spb.ap[:, 0:128], func=AF.Exp), r=[spb.n], w=[pt.n + ".a"])
                        if n > 128:
                            P.act(lambda e: e.activation(out=pt.ap[:, 128:n], in_=bank(sb)[:, 128:n], func=AF.Exp, scale=float(scale)),
                                  r=[], w=[psn(sb), pt.n + ".b"])
                    else:
                        P.act(lambda e: e.activation(out=pt.ap[:, 0:n], in_=bank(sb)[:, 0:n], func=AF.Exp, scale=float(scale)),
                              r=[], w=[psn(sb), pt.n + ".a", pt.n + ".b"])
                return pt

            def emit_pv(si, pt):
                h, qg, kb, nkb, q0, n, c0 = geom(steps[si])
                hb = h % 2
                ob = 2 + 2 * (qg % 2)
                first = (kb == 0); last = (kb == nkb - 1)

                def f_pv(e):
                    e.matmul(bank(ob)[:, c0:c0 + n], lhsT=vv[hb].ap[:, kb, :], rhs=pt.ap[:, 0:n], start=first, stop=last, skip_group_check=True)
                    return e.matmul(bank(ob + 1)[:, c0:c0 + n], lhsT=ones_bf.ap, rhs=pt.ap[:, 0:n], start=first, stop=last, skip_group_check=True)
                P.pe(f_pv, r=[pt.n, pt.n + ".a", pt.n + ".b", vv[hb].n, ones_bf.n], w=[psn(ob), psn(ob + 1)])
                if last:
                    P.act(lambda e: e.activation(out=tt3.ap, in_=bank(ob + 1), func=AF.Ln), r=[], w=[psn(ob + 1), tt3.n])
                    P.act(lambda e: e.activation(out=rl.ap, in_=tt3.ap, func=AF.Exp, scale=-1.0), r=[tt3.n], w=[rl.n])
                    P.dve(lambda e: e.tensor_tensor(out=onT.ap[:, h, qg * 512:(qg + 1) * 512], in0=bank(ob), in1=rl.ap, op=ALU.mult),
                          r=[rl.n], w=[psn(ob), onT.n + ".%d.%d" % (h, qg)])

            SBK = [0, 1, 6, 7]
            LA = 3
            head_loads(0)
            for j in range(LA):
                emit_qk(j)
            for si in range(len(steps)):
                h, qg, kb, nkb = steps[si]
                if qg == 0 and kb == 0 and h + 1 < 8:
                    head_loads(h + 1)
                pt = emit_sm(si)
                if si + LA < len(steps):
                    emit_qk(si + LA)
                emit_pv(si, pt)
            if grp == 1:
                P.barrier()
            for qg in range(4):
                for h in range(8):
                    s = h % 2
                    P.act(lambda e, h=h, qg=qg, s=s: e.activation(out=sq3[s].ap, in_=onT.ap[:, h, qg * 512:(qg + 1) * 512], func=AF.Square),
                          r=[onT.n + ".%d.%d" % (h, qg)], w=[sq3[s].n])
                    P.pe(lambda e, h=h, s=s: e.matmul(bank(0), lhsT=ones_bf.ap, rhs=sq3[s].ap, start=(h == 0), stop=(h == 7)),
                         r=[sq3[s].n, ones_bf.n], w=[psn(0)])
                P.act(lambda e: e.activation(out=tt3.ap, in_=bank(0), func=AF.Sqrt, scale=1.0 / 1024, bias=epsb.ap), r=[epsb.n], w=[psn(0), tt3.n])
                P.dve(lambda e: e.reciprocal(out=rsb.ap, in_=tt3.ap), r=[tt3.n], w=[rsb.n])
                for h in range(8):
                    P.dve(lambda e, h=h, qg=qg, grp=grp: e.scalar_tensor_tensor(
                        out=onrm[grp].ap[:, h, qg * 512:(qg + 1) * 512], in0=onT.ap[:, h, qg * 512:(qg + 1) * 512],
                        scalar=gcol.ap[:, grp * 8 + h:grp * 8 + h + 1], in1=rsb.ap, op0=ALU.mult, op1=ALU.mult),
                        r=[onT.n + ".%d.%d" % (h, qg), rsb.n, gcol.n + "%d" % grp], w=[onrm[grp].n + ".%d" % qg])
            if grp == 1:
                P.barrier()
        if stop <= 3:
            P.dma(lambda e: e.dma_start(out=onT_d[:, 0:8, :], in_=onrm[0].ap), r=[], w=["onT_d"])
            P.dma(lambda e: e.dma_start(out=onT_d[:, 8:16, :], in_=onrm[1].ap), r=[], w=["onT_d"])
            return finish(nc, P, ["onT_d"])

        cc = Bump(AR, C0, END, "C4")
        wo = [cc.alloc("wo%d" % i, [128, 16, 512], BF16) for i in range(2)]
        xt = [cc.alloc("xt%d" % i, [128, 512], F32) for i in range(2)]
        ht = [cc.alloc("ht%d" % i, [128, 512], F32) for i in range(2)]
        onrm_all = [onrm[0].n + ".%d" % q for q in range(4)] + [onrm[1].n + ".%d" % q for q in range(4)]
        for dg in range(4):
            w = wo[dg % 2]
            load_w(w, w_o, dg * 512, 512, 16)
            for c in range(NT):
                s = c % 2
                P.dma(lambda e, c=c, dg=dg, s=s: e.dma_start(out=xt[s].ap, in_=x[c * 128:(c + 1) * 128, dg * 512:(dg + 1) * 512]), w=[xt[s].n])
                bk = next_bank()
                P.pe(lambda e, c=c, w=w, bk=bk: [e.matmul(bank(bk), lhsT=onrm[k // 8].ap[:, k % 8, c * 128:(c + 1) * 128], rhs=w.ap[:, k, :],
                                                          start=(k == 0), stop=(k == 15)) for k in range(16)],
                     r=onrm_all + [w.n], w=[psn(bk)])
                P.dve(lambda e, bk=bk, s=s: e.tensor_tensor(out=ht[s].ap, in0=bank(bk), in1=xt[s].ap, op=ALU.add),
                      r=[xt[s].n], w=[psn(bk), ht[s].n])
                P.dma(lambda e, c=c, dg=dg, s=s: e.dma_start(out=h2_d[c * 128:(c + 1) * 128, dg * 512:(dg + 1) * 512], in_=ht[s].ap),
                      r=[ht[s].n], w=["h2_d"], q="act")
        P.barrier()
        if stop <= 4:
            return finish(nc, P, [])
        xn2T = AR.view("A.xT", A0, [128, 16, S], BF16)
        norm_T("n2", h2_d, ln2_g, xn2T)
        P.barrier()
        P.dma(lambda e: e.dma_start(out=xT_d, in_=xn2T.ap), r=[], w=["xT_d"])

        qp = AR.view("B.qp", B0, [128, 16, S], BF16)
        cc = Bump(AR, C0, END, "C5a")
        wq = [cc.alloc("wq%d" % i, [128, 16, 512], BF16) for i in range(2)]
        kn = cc.alloc("kn", [128, 16, 128], BF16)
        P.dma(lambda e: e.dma_start(out=kn.ap, in_=sub_keys.rearrange("g n c -> n g c")), w=[kn.n], q="pool")
        P.pe(lambda e: [e.transpose(out=bankb(0, 2)[:, g * 128:(g + 1) * 128], in_=kn.ap[:, g, :], identity=ident_bf.ap) for g in range(16)],
             r=[kn.n, ident_bf.n], w=[psn(0), psn(1)])
        evac(keysT.ap, bankb(0, 2).rearrange("p (g n) -> p g n", n=128), [], [psn(0), psn(1), keysT.n])
        x2r = xT_reads(xn2T)
        for blk in range(4):
            w = wq[blk % 2]
            load_w(w, peer_wq, blk * 512, 512, 16)
            for m in range(4):
                g = blk * 4 + m
                for tg in range(4):
                    bk = next_bank()
                    mm_fm(w, m * 128, 128, xn2T, 16, tg, bk, x2r + [w.n])
                    evac(qp.ap[:, g, tg * 512:(tg + 1) * 512], bank(bk), [], [psn(bk), qp.n + ".%d" % g])
        cc5 = Bump(AR, C0 + 40 * KB, END, "C5s")
        ssc = [cc5.alloc("ssc%d" % i, [128, S], F32) for i in range(2)]
        for c in range(NT):
            pb = 4 * (c % 2)
            for g in range(16):
                P.pe(lambda e, g=g, c=c, pb=pb: e.matmul(bank(pb + g // 4)[:, (g % 4) * 128:(g % 4 + 1) * 128], lhsT=qp.ap[:, g, c * 128:(c + 1) * 128],
                                                         rhs=keysT.ap[:, g, :], start=True, stop=True, skip_group_check=True),
                     r=[qp.n + ".%d" % g, keysT.n], w=[psn(pb + g // 4)])
            for b4 in range(4):
                evac(ssc[c % 2].ap[:, b4 * 512:(b4 + 1) * 512], bank(pb + b4), [], [psn(pb + b4), ssc[c % 2].n])
            P.dma(lambda e, c=c: e.dma_start(out=sc_d[c], in_=ssc[c % 2].ap), r=[ssc[c % 2].n], w=["sc_d"], q="act")
        P.barrier()
        if stop <= 5:
            return finish(nc, P, ["xT_d"])

        PB = PART_BOUNDS
        NP = len(PB) - 1
        def part_of(c): return max(p for p in range(NP) if PB[p] <= c)
        def nch(p): return PB[p + 1] - PB[p]
        def tok0(p): return PB[p] * 128
        def ntok(p): return nch(p) * 128
        CPPo = max([nch(p) for p in range(NP - 1)] + [4])
        TPo = CPPo * 128
        ab = Bump(AR, A0, END, "X5")
        ssb2 = [ab.alloc("ssb%d" % i, [128, 16, 128], F32) for i in range(2)]
        tv = ab.alloc("tv", [128, 16, 16], F32); ti = ab.alloc("ti", [128, 16, 16], U32); tif = ab.alloc("tif", [128, 16, 16], F32)
        cand = ab.alloc("cand", [128, 8, 256], F32)
        cv = ab.alloc("cv", [128, 8, 16], F32); ci = ab.alloc("ci", [128, 8, 16], U32)
        aku = ab.alloc("aku", [128, 8, 16], U32); bku = ab.alloc("bku", [128, 8, 16], U32)
        akf = ab.alloc("akf", [128, 8, 16], F32); bkf = ab.alloc("bkf", [128, 8, 16], F32)
        oh = [ab.alloc("oh%d" % i, [128, 8, 16, 16], F32) for i in range(2)]
        ik = ab.alloc("ik", [128, 128], F32); jk = ab.alloc("jk", [128, 128], F32); gk = ab.alloc("gk", [128, 128], F32)
        dd = ab.alloc("dd", [128, 8, 16], F32); ee = ab.alloc("ee", [128, 8, 16], F32); zz = ab.alloc("zz", [128, 8], F32); rz = ab.alloc("rz", [128, 8], F32)
        tT = ab.alloc("tT", [128, 3, 128], F32)
        PQ = [(ab.alloc("P%d" % i, [128, 16, 128], BF16), ab.alloc("Q%d" % i, [128, 16, 128], BF16)) for i in range(3)]
        Gc = ab.alloc("Gc", [128, 128, 128], BF16)
        xh = ab.alloc("xh", [128, 16, TPo], BF16)
        Gg = [ab.alloc("Gg%d" % i, [128, CPPo, 4, 128], BF16) for i in range(2)]
        Un = [ab.alloc("Un%d" % i, [128, 2, DM], BF16) for i in range(3)]
        UT = [ab.alloc("UT%d" % i, [128, 16, 128], BF16) for i in range(2)]
        Wt = [ab.alloc("Wt%d" % i, [128, TPo], BF16) for i in range(2)]
        ga = [ab.alloc("ga%d" % i, [128, 512], BF16) for i in range(2)]
        tvv = tv.ap.rearrange("p (h s) k -> p h s k", s=2)
        tifv = tif.ap.rearrange("p (h s) k -> p h s k", s=2)
        candv = cand.ap.rearrange("p h (a b) -> p h a b", b=16)
        iota16v = iota16.ap.unsqueeze(1).unsqueeze(1).broadcast_to([128, 8, 16, 16])
        pqi = [0]
        gbi = [0]

        def stage_A(c, pieces=None):
            sb_ = ssb2[c % 2]
            if pieces is None or 0 in pieces:
                P.dma(lambda e: e.dma_start(out=sb_.ap.rearrange("p g n -> p (g n)"), in_=sc_d[c]), r=["sc_d"], w=[sb_.n + ".ld"])
            for g in range(16):
                if pieces is not None and (g // 2) not in pieces:
                    continue
                tn = tv.n + ".%d" % g
                sn = sb_.n + ".g%d" % g
                P.dve(lambda e, g=g: e.max(out=tv.ap[:, g, 0:8], in_=sb_.ap[:, g, :]), r=[sb_.n + ".ld", sn], w=[tn + "a"])
                P.dve(lambda e, g=g: e.max_index(out=ti.ap[:, g, 0:8], in_max=tv.ap[:, g, 0:8], in_values=sb_.ap[:, g, :]), r=[sb_.n + ".ld", sn, tn + "a"], w=[tn + "ia"])
                P.dve(lambda e, g=g: e.match_replace(out=sb_.ap[:, g, :], in_to_replace=tv.ap[:, g, 0:8], in_values=sb_.ap[:, g, :], imm_value=-1e30),
                      r=[sb_.n + ".ld", tn + "a"], w=[sn])
                P.dve(lambda e, g=g: e.max(out=tv.ap[:, g, 8:16], in_=sb_.ap[:, g, :]), r=[sb_.n + ".ld", sn], w=[tn + "b"])
                P.dve(lambda e, g=g: e.max_index(out=ti.ap[:, g, 8:16], in_max=tv.ap[:, g, 8:16], in_values=sb_.ap[:, g, :]), r=[sb_.n + ".ld", sn, tn + "b"], w=[tn + "ib"])

        def stage_B(c):
            tvn = [tv.n + ".%d%s" % (g, s_) for g in range(16) for s_ in "ab"]
            tin = [tv.n + ".%d%s" % (g, s_) for g in range(16) for s_ in ("ia", "ib")]
            P.dve(lambda e: e.tensor_copy(out=tif.ap, in_=ti.ap), r=tin, w=[tif.n])
            P.dve(lambda e: e.tensor_tensor(out=candv, in0=tvv[:, :, 0, :].unsqueeze(3).broadcast_to([128, 8, 16, 16]),
                                            in1=tvv[:, :, 1, :].unsqueeze(2).broadcast_to([128, 8, 16, 16]), op=ALU.add),
                  r=tvn, w=[cand.n])
            for h in range(8):
                cn = cv.n + ".%d" % h
                P.dve(lambda e, h=h: e.max(out=cv.ap[:, h, 0:8], in_=cand.ap[:, h, :]), r=[cand.n], w=[cn + "a"])
                P.dve(lambda e, h=h: e.max_index(out=ci.ap[:, h, 0:8], in_max=cv.ap[:, h, 0:8], in_values=cand.ap[:, h, :]), r=[cand.n, cn + "a"], w=[cn + "ia"])
                P.dve(lambda e, h=h: e.match_replace(out=cand.ap[:, h, :], in_to_replace=cv.ap[:, h, 0:8], in_values=cand.ap[:, h, :], imm_value=-1e30),
                      r=[cn + "a"], w=[cand.n])
                P.dve(lambda e, h=h: e.max(out=cv.ap[:, h, 8:16], in_=cand.ap[:, h, :]), r=[cand.n], w=[cn + "b"])
                P.dve(lambda e, h=h: e.max_index(out=ci.ap[:, h, 8:16], in_max=cv.ap[:, h, 8:16], in_values=cand.ap[:, h, :]), r=[cand.n, cn + "b"], w=[cn + "ib"])
            cvn = [cv.n + ".%d%s" % (h, s_) for h in range(8) for s_ in "ab"]
            cin = [cv.n + ".%d%s" % (h, s_) for h in range(8) for s_ in ("ia", "ib")]
            P.dve(lambda e: e.tensor_tensor(out=dd.ap, in0=cv.ap, in1=cv.ap[:, :, 0:1].broadcast_to([128, 8, 16]), op=ALU.subtract), r=cvn, w=[dd.n])
            P.act(lambda e: e.activation(out=ee.ap, in_=dd.ap, func=AF.Exp), r=[dd.n], w=[ee.n])
            P.dve(lambda e: e.tensor_single_scalar(out=aku.ap, in_=ci.ap, scalar=4, op=ALU.logical_shift_right), r=cin, w=[aku.n])
            P.dve(lambda e: e.tensor_single_scalar(out=bku.ap, in_=ci.ap, scalar=15, op=ALU.bitwise_and), r=cin, w=[bku.n])
            P.dve(lambda e: e.tensor_copy(out=akf.ap, in_=aku.ap), r=[aku.n], w=[akf.n])
            P.dve(lambda e: e.tensor_copy(out=bkf.ap, in_=bku.ap), r=[bku.n], w=[bkf.n])
            for side, (sel, dst) in enumerate([(akf, ik), (bkf, jk)]):
                o0, o1 = oh
                P.dve(lambda e, sel=sel, o0=o0: e.tensor_tensor(out=o0.ap, in0=sel.ap.unsqueeze(3).broadcast_to([128, 8, 16, 16]), in1=iota16v, op=ALU.is_equal),
                      r=[sel.n, iota16.n], w=[o0.n])
                P.dve(lambda e, side=side, o0=o0, o1=o1: e.tensor_tensor(out=o1.ap, in0=o0.ap, in1=tifv[:, :, side, :].unsqueeze(2).broadcast_to([128, 8, 16, 16]), op=ALU.mult),
                      r=[o0.n, tif.n], w=[o1.n])
                P.dve(lambda e, dst=dst, o1=o1: e.tensor_reduce(out=dst.ap.rearrange("p (h k) -> p h k", k=16), in_=o1.ap, axis=AX.X, op=ALU.add),
                      r=[o1.n], w=[dst.n])
            P.dve(lambda e: e.tensor_reduce(out=zz.ap, in_=ee.ap, axis=AX.X, op=ALU.add), r=[ee.n], w=[zz.n])
            P.dve(lambda e: e.reciprocal(out=rz.ap, in_=zz.ap), r=[zz.n], w=[rz.n])
            P.dve(lambda e: e.tensor_tensor(out=gk.ap.rearrange("p (h k) -> p h k", k=16), in0=ee.ap, in1=rz.ap.unsqueeze(2).broadcast_to([128, 8, 16]), op=ALU.mult),
                  r=[ee.n, rz.n], w=[gk.n])

        def stage_Bpe(c):
            P.pe(lambda e: [e.transpose(out=bank(0)[:, i * 128:(i + 1) * 128], in_=src.ap, identity=ident_f.ap) for i, src in enumerate([ik, jk, gk])],
                 r=[ik.n, jk.n, gk.n, ident_f.n], w=[psn(0)])
            P.act(lambda e: e.activation(out=tT.ap, in_=bank(0)[:, 0:384].rearrange("p (a t) -> p a t", t=128), func=AF.Copy), r=[], w=[psn(0), tT.n])

        def stage_C_group(c, t0):
            Pb, Qb = PQ[pqi[0] % len(PQ)]; pqi[0] += 1

            def f_q(e):
                return e.tensor_tensor(out=Qb.ap, in0=iota_bf.ap.unsqueeze(1).broadcast_to([128, 16, 128]),
                                       in1=tT.ap[:, 1, t0:t0 + 16].unsqueeze(2).broadcast_to([128, 16, 128]), op=ALU.is_equal)

            def f_p(e):
                for tt in range(16):
                    i = e.tensor_scalar(out=Pb.ap[:, tt, :], in0=iota_bf.ap, scalar1=tT.ap[:, 0, t0 + tt:t0 + tt + 1],
                                        scalar2=tT.ap[:, 2, t0 + tt:t0 + tt + 1], op0=ALU.is_equal, op1=ALU.mult)
                return i
            P.dve(f_q, r=[tT.n, iota_bf.n], w=[Qb.n])
            P.dve(f_p, r=[tT.n, iota_bf.n], w=[Pb.n])
            for sub in range(4):
                gb = 1 + gbi[0] % 3; gbi[0] += 1

                def f_g(e, sub=sub, gb=gb):
                    for u in range(4):
                        tt = sub * 4 + u
                        i = e.matmul(bank(gb).rearrange("p (i t) -> p i t", t=4)[:, :, u], lhsT=Qb.ap[:, tt, :], rhs=Pb.ap[:, tt, :],
                                     start=True, stop=True, skip_group_check=True)
                    return i
                P.pe(f_g, r=[Pb.n, Qb.n], w=[psn(gb)])
                ts = t0 + sub * 4
                P.act(lambda e, gb=gb, ts=ts: e.activation(out=Gc.ap[:, :, ts:ts + 4],
                                                           in_=bank(gb).rearrange("p (i t) -> p i t", t=4), func=AF.Copy),
                      r=[], w=[psn(gb), Gc.n])

        def stage_C_store(c):
            P.dma(lambda e: e.dma_start(out=G_d[c].rearrange("j i t -> j (i t)"), in_=Gc.ap.rearrange("j i t -> j (i t)")),
                  r=[Gc.n], w=["G_d.%d" % part_of(c)], q="act")

        G_v = G_d.rearrange("c j i t -> j c i t")
        U_v2 = peer_u.rearrange("(g ii j) d -> g j ii d", ii=2, j=128)
        w_state = {"ga": 0, "mm": 0}

        WB = {"xh": xh, "Gg": Gg, "Un": Un, "UT": UT, "Wt": Wt, "ga": ga}

        def w_group_loads(p, ig):
            Gg_ = WB["Gg"]; s = ig % len(Gg_)
            P.dma(lambda e: e.dma_start(out=Gg_[s].ap[:, 0:nch(p)], in_=G_v[:, PB[p]:PB[p + 1], 4 * ig:4 * ig + 4, :]), r=["G_d.%d" % p], w=[Gg_[s].n])

        def w_U_load(q):
            Un_ = WB["Un"]; s = q % len(Un_)
            P.dma(lambda e: e.dma_start(out=Un_[s].ap, in_=U_v2[q]), w=[Un_[s].n], q="pool")

        def w_part_begin(p):
            xh_ = WB["xh"]
            P.dma(lambda e: e.dma_start(out=xh_.ap[:, :, 0:ntok(p)], in_=xT_d[:, :, tok0(p):tok0(p) + ntok(p)]), r=["xT_d"], w=[xh_.n])
            w_group_loads(p, 0)
            if p == 0:
                w_U_load(0); w_U_load(1)
            if p > 0:
                for i in range(len(WB["UT"]) - 1):
                    w_UT_load(i)

        def w_UT_load(i):
            UT_ = WB["UT"]; u = i % len(UT_)
            P.dma(lambda e: e.dma_start(out=UT_[u].ap, in_=UT_d[i]), r=["UT_d"], w=[UT_[u].n])

        def w_T(i):
            ii = i % 2
            Un_ = WB["Un"]; UT_ = WB["UT"]
            s = (i // 2) % len(Un_)
            u = i % len(UT_)
            P.pe(lambda e: [e.transpose(out=bankb(4, 2)[:, k * 128:(k + 1) * 128], in_=Un_[s].ap[:, ii, k * 128:(k + 1) * 128],
                                        identity=ident_bf.ap) for k in range(16)],
                 r=[Un_[s].n, ident_bf.n], w=[psn(4), psn(5)])
            P.act(lambda e: e.activation(out=UT_[u].ap, in_=bankb(4, 2).rearrange("p (k j) -> p k j", j=128), func=AF.Copy),
                  r=[], w=[psn(4), psn(5), UT_[u].n])
            if NP > 1:
                P.dma(lambda e: e.dma_start(out=UT_d[i], in_=UT_[u].ap), r=[UT_[u].n], w=["UT_d"], q="act")

        def w_block(p, i):
            ig, ii = i // 4, i % 4
            xh_ = WB["xh"]; Gg_ = WB["Gg"]; UT_ = WB["UT"]; Wt_ = WB["Wt"]; ga_ = WB["ga"]
            s = ig % len(Gg_)
            u = i % len(UT_)
            wu = i % len(Wt_)
            if ii == 0 and ig + 1 < 32:
                w_group_loads(p, ig + 1)
            if p == 0:
                if i % 2 == 0 and i // 2 + 2 < 64:
                    w_U_load(i // 2 + 2)
                if i == 0:
                    w_T(0)
                if i + 1 < 128:
                    w_T(i + 1)
            else:
                nxt = i + len(UT_) - 1
                if nxt < 128:
                    w_UT_load(nxt)
            for tg in range(ntok(p) // 512):
                bks = WB.get("banks", [6, 7])
                bk = bks[w_state["mm"] % len(bks)]; w_state["mm"] += 1
                P.pe(lambda e, tg=tg, bk=bk: [e.matmul(bank(bk), lhsT=UT_[u].ap[:, k, :], rhs=xh_.ap[:, k, tg * 512:(tg + 1) * 512],
                                                       start=(k == 0), stop=(k == 15)) for k in range(16)],
                     r=[UT_[u].n, xh_.n], w=[psn(bk)])
                gb_ = ga_[w_state["ga"] % len(ga_)]; w_state["ga"] += 1
                P.act(lambda e, bk=bk, gb_=gb_: e.activation(out=gb_.ap, in_=bank(bk), func=AF.Gelu), r=[], w=[psn(bk), gb_.n])
                P.pool(lambda e, tg=tg, gb_=gb_: e.tensor_tensor(
                    out=Wt_[wu].ap[:, tg * 512:(tg + 1) * 512].rearrange("p (c t) -> p c t", t=128),
                    in0=gb_.ap.rearrange("p (c t) -> p c t", t=128), in1=Gg_[s].ap[:, 4 * tg:4 * tg + 4, ii, :], op=ALU.mult),
                    r=[gb_.n, Gg_[s].n], w=[Wt_[wu].n])
            P.dma(lambda e: e.dma_start(out=W_d[i][:, tok0(p):tok0(p) + ntok(p)], in_=Wt_[wu].ap[:, 0:ntok(p)]), r=[Wt_[wu].n], w=["W_d"], q="pool")

        wnext = {"p": -1, "i": 0}

        def w_emit(n):
            for _ in range(n):
                if wnext["p"] >= 0 and wnext["i"] < 128:
                    w_block(wnext["p"], wnext["i"]); wnext["i"] += 1

        stage_A(0); stage_B(0); stage_Bpe(0)
        for c in range(NT):
            part = part_of(c)
            n_round = 0
            if part >= 1:
                if c == PB[part]:
                    w_part_begin(part - 1)
                    wnext["p"] = part - 1; wnext["i"] = 0
                r_ = c - PB[part] + 1
                n_round = -(-128 * r_ // nch(part)) - wnext["i"]
            n_slots = n_round
            done_ = 0
            for k, t0 in enumerate(range(0, 128, 16)):
                stage_C_group(c, t0)
                tgt = n_slots * (k + 1) // 8
                w_emit(tgt - done_); done_ = tgt
                if c + 1 < NT:
                    stage_A(c + 1, pieces=(k,))
            stage_C_store(c)
            rest = n_round - n_slots
            if c + 1 < NT:
                stage_B(c + 1)
                w_emit(rest * 3 // 4)
                stage_Bpe(c + 1)
                w_emit(rest - rest * 3 // 4)
            else:
                w_emit(rest)
        assert NP == 1 or wnext["i"] == 128, wnext
        if NP > 1:
            P.barrier()
            fb = Bump(AR, A0, END, "X5f")
            TPf = ntok(NP - 1)
            WB = {"xh": fb.alloc("xh", [128, 16, TPf], BF16),
                  "Gg": [fb.alloc("Gg%d" % i, [128, nch(NP - 1), 4, 128], BF16) for i in range(2)],
                  "Un": None,
                  "UT": [fb.alloc("UT%d" % i, [128, 16, 128], BF16) for i in range(6)],
                  "Wt": [fb.alloc("Wt%d" % i, [128, TPf], BF16) for i in range(3)],
                  "ga": [fb.alloc("ga%d" % i, [128, 512], BF16) for i in range(4)],
                  "banks": [2, 3, 4, 5, 6, 7]}
        w_part_begin(NP - 1)
        for i in range(128):
            w_block(NP - 1, i)
        P.barrier()
        if stop <= 7:
            return finish(nc, P, ["W_d", "G_d"])

        hf = AR.view("A.hf", A0, [128, 8, DM], F32)
        bb = Bump(AR, B0, C0, "B5d"); cc = Bump(AR, C0, END, "C5d")
        NB5 = 4
        Wg = [bb.alloc("Wg%d" % i, [128, 4, 1024], BF16) for i in range(NB5)]
        Vg = [bb.alloc("Vg%d" % i, [128, 4, 512], BF16) for i in range(NB5)]
        h2t = [cc.alloc("h2t%d" % i, [128, 512], F32) for i in range(8)]
        gfr = cc.alloc("gfr", [128, DM], F32)
        ot = [cc.alloc("ot%d" % i, [128, DM], F32) for i in range(2)]
        sq5 = cc.alloc("sq5", [128, DM], BF16)
        P.dma(lambda e: e.dma_start(out=gfr.ap, in_=lnf_g.broadcast_to([128, DM])), w=[gfr.n])
        W_v = W_d.rearrange("(g ii) j t -> g j ii t", ii=4)
        V_v = peer_v.rearrange("(g ii j) d -> g j ii d", ii=4, j=128)
        li = [0]
        for th in range(2):
            for dg in range(4):
                for ig in range(32):
                    if ig == 12:
                        for c8 in range(8):
                            cg = th * 8 + c8
                            P.dma(lambda e, cg=cg, dg=dg, c8=c8: e.dma_start(out=h2t[c8].ap, in_=h2_d[cg * 128:(cg + 1) * 128, dg * 512:(dg + 1) * 512]), r=["h2_d"], w=[h2t[c8].n])
                    s = li[0] % NB5; li[0] += 1
                    P.dma(lambda e, ig=ig, s=s, th=th: e.dma_start(out=Wg[s].ap, in_=W_v[ig][:, :, th * 1024:(th + 1) * 1024]), r=["W_d"], w=[Wg[s].n])
                    P.dma(lambda e, ig=ig, s=s, dg=dg: e.dma_start(out=Vg[s].ap, in_=V_v[ig][:, :, dg * 512:(dg + 1) * 512]), w=[Vg[s].n], q="pool")

                    def f_y(e, ig=ig, s=s):
                        for ii in range(4):
                            i = 4 * ig + ii
                            for c8 in range(8):
                                r_ = e.matmul(bank(c8), lhsT=Wg[s].ap[:, ii, c8 * 128:(c8 + 1) * 128], rhs=Vg[s].ap[:, ii, :],
                                              start=(i == 0), stop=(i == 127), skip_group_check=True)
                        return r_
                    P.pe(f_y, r=[Wg[s].n, Vg[s].n], w=[psn(b) for b in range(8)])
                for c8 in range(8):
                    P.dve(lambda e, c8=c8, dg=dg: e.tensor_tensor(out=hf.ap[:, c8, dg * 512:(dg + 1) * 512], in0=bank(c8), in1=h2t[c8].ap, op=ALU.add),
                          r=[h2t[c8].n], w=[psn(c8), hf.n + ".%d" % c8])
            for c8 in range(8):
                cg = th * 8 + c8
                s = c8 % 2
                nm = ["5.ss%d" % cg, "5.t%d" % cg, "5.rs%d" % cg]
                P.act(lambda e, c8=c8, cg=cg: e.activation(out=sq5.ap, in_=hf.ap[:, c8, :], func=AF.Square, accum_out=ssA.ap[:, cg:cg + 1]),
                      r=[hf.n + ".%d" % c8], w=[sq5.n, nm[0]])
                rstd_ops(ssA.ap[:, cg:cg + 1], tA.ap[:, cg:cg + 1], rsA.ap[:, cg:cg + 1], nm, DM)
                P.dve(lambda e, c8=c8, cg=cg, s=s: e.scalar_tensor_tensor(out=ot[s].ap, in0=hf.ap[:, c8, :], scalar=rsA.ap[:, cg:cg + 1], in1=gfr.ap,
                                                                          op0=ALU.mult, op1=ALU.mult),
                      r=[hf.n + ".%d" % c8, nm[2], gfr.n], w=[ot[s].n])
                P.dma(lambda e, cg=cg, s=s: e.dma_start(out=out[cg * 128:(cg + 1) * 128, :], in_=ot[s].ap), r=[ot[s].n], w=["out"], q="act")
        return finish(nc, P, ["out"])


def finish(nc, P, outs):
    P.add("sp", lambda e: None, reads=list(outs) + ["out"])
    P.barrier()
    P.add("sp", lambda e: None)
    P.emit()
    return nc


_CONSTS = None


def make_in_maps(inputs):
    global _CONSTS
    if _CONSTS is None:
        _CONSTS = host_constants()
    f = lambda a: np.ascontiguousarray(np.asarray(a, dtype=np.float32))
    shared = {
        "ln1_g": f(inputs["ln1_g"]).reshape(1, DM), "w_in": f(inputs["w_in"]).reshape(DM, 4160),
        "q_a_norm": f(inputs["q_a_norm"]).reshape(1, 512), "kv_a_norm": f(inputs["kv_a_norm"]).reshape(1, 512),
        "w_uq": f(inputs["w_uq"]).reshape(512, 1536), "w_ukv": f(inputs["w_ukv"]).reshape(512, 2048),
        "out_norm_dil": f(inputs["out_norm_dil"]).reshape(1, 1024), "out_norm_mla": f(inputs["out_norm_mla"]).reshape(1, 1024),
        "w_o": f(inputs["w_o"]).reshape(DM, DM), "ln2_g": f(inputs["ln2_g"]).reshape(1, DM),
        "peer_wq": f(inputs["peer_wq"]).reshape(DM, DM), "peer_sub_keys": f(inputs["peer_sub_keys"]).reshape(16, 128, 128),
        "peer_u": f(inputs["peer_u"]).reshape(16384, DM), "peer_v": f(inputs["peer_v"]).reshape(16384, DM),
        "lnf_g": f(inputs["lnf_g"]).reshape(1, DM),
    }
    shared.update(_CONSTS)
    xx = f(inputs["x"])
    maps = []
    for b in range(8):
        m = dict(shared)
        m["x"] = xx[b]
        maps.append(m)
    return maps


def kernel(**inputs):
    nc = build_program()
    in_maps = make_in_maps(inputs)
    res = run_bass_kernel_spmd(nc, in_maps, core_ids=list(range(8)))
    return np.stack([np.asarray(r["out"], dtype=np.float32) for r in res.results], 0)
```
